# Optimizing a Trainium2 kernel written in Bass

```python
import math
import jax, jax.numpy as jnp
from jax import lax
import numpy as np

D_MODEL = 1024
BATCH = 4
SEQ = 4096
DEPTH = 1

HEAD_DIM = 64
FOX_HEADS = 8
FOX_WIDTH = FOX_HEADS * HEAD_DIM
DIFF_HEADS = 4
DIFF_QK_DIM = HEAD_DIM
DIFF_QK_WIDTH = DIFF_HEADS * 2 * DIFF_QK_DIM
DIFF_V_DIM = 2 * HEAD_DIM
DIFF_WIDTH = DIFF_HEADS * DIFF_V_DIM
MEM_HEADS = 4
MEM_HEAD_DIM = 128
MEM_WIDTH = MEM_HEADS * MEM_HEAD_DIM
N_MEM = 256
N_BRANCH = 3
Q_BLOCK = 128
N_GROUPS = 4
EXPERTS_PER_GROUP = 8
N_EXPERTS = N_GROUPS * EXPERTS_PER_GROUP
TOP_K_INNER = 2
D_EXPERT = 256
LN_EPS = 1e-5
FORGET_BIAS_OFFSET = 2.0
DEEPNORM_ALPHA = (2.0 * DEPTH) ** 0.25
DEEPNORM_BETA = (8.0 * DEPTH) ** -0.25
IN_SPLITS = (FOX_WIDTH, FOX_WIDTH, FOX_WIDTH,
             DIFF_QK_WIDTH, DIFF_QK_WIDTH, DIFF_WIDTH,
             MEM_WIDTH,
             N_BRANCH * D_MODEL,
             FOX_HEADS)
IN_COLS = sum(IN_SPLITS)
IN_OFFSETS = [int(o) for o in np.cumsum(IN_SPLITS)[:-1]]

kernel_name = "hybrid_fox_diff_mem_hmoe_deepnorm"


def layer_norm(x, g, b):
    xf = x.astype(jnp.float32)
    mu = jnp.mean(xf, axis=-1, keepdims=True)
    var = jnp.mean(jnp.square(xf - mu), axis=-1, keepdims=True)
    return ((xf - mu) * lax.rsqrt(var + LN_EPS) * g + b).astype(x.dtype)


def rms_norm(x, g):
    xf = x.astype(jnp.float32)
    return (xf * lax.rsqrt(jnp.mean(jnp.square(xf), axis=-1, keepdims=True) + LN_EPS) * g).astype(x.dtype)


def _to_blocks(a):
    b, h, s = a.shape[:3]
    a = a.reshape((b, h, s // Q_BLOCK, Q_BLOCK) + a.shape[3:])
    return jnp.moveaxis(a, 2, 0)


def _from_blocks(o):
    nb, b, h, blk, dv = o.shape
    o = jnp.moveaxis(o, 0, 2).reshape(b, h, nb * blk, dv)
    return jnp.transpose(o, (0, 2, 1, 3))


def fox_attention(q, k, v, log_f):
    s_len = q.shape[2]
    c = jnp.cumsum(log_f, axis=-1)
    pos = jnp.arange(s_len)
    scale = HEAD_DIM ** -0.5

    def block(args):
        qb, cb, pb = args
        logits = jnp.einsum('bhqd,bhkd->bhqk', qb, k).astype(jnp.float32) * scale
        logits = logits + (cb[..., None] - c[:, :, None, :])
        causal = pb[:, None] >= pos[None, :]
        p = jax.nn.softmax(jnp.where(causal, logits, -jnp.inf), axis=-1)
        return jnp.einsum('bhqk,bhkd->bhqd', p.astype(v.dtype), v)

    out = lax.map(block, (_to_blocks(q), _to_blocks(c), pos.reshape(-1, Q_BLOCK)))
    return _from_blocks(out)


def diff_attention(q1, q2, k1, k2, v, lam, slopes):
    s_len = q1.shape[2]
    pos = jnp.arange(s_len)
    scale = DIFF_QK_DIM ** -0.5

    def block(args):
        q1b, q2b, pb = args
        dist = (pb[:, None] - pos[None, :]).astype(jnp.float32)
        bias = -slopes[:, None, None] * dist
        causal = dist >= 0

        def probs(qb, kk):
            lg = jnp.einsum('bhqd,bhkd->bhqk', qb, kk).astype(jnp.float32) * scale + bias
            return jax.nn.softmax(jnp.where(causal, lg, -jnp.inf), axis=-1)

        p = probs(q1b, k1) - lam * probs(q2b, k2)
        return jnp.einsum('bhqk,bhkd->bhqd', p.astype(v.dtype), v)

    out = lax.map(block, (_to_blocks(q1), _to_blocks(q2), pos.reshape(-1, Q_BLOCK)))
    return _from_blocks(out)


def memory_attention(q, km, vm):
    logits = jnp.einsum('bhqd,bhmd->bhqm', q, km).astype(jnp.float32) * (MEM_HEAD_DIM ** -0.5)
    p = jax.nn.softmax(logits, axis=-1)
    out = jnp.einsum('bhqm,bhmd->bhqd', p.astype(vm.dtype), vm)
    return jnp.transpose(out, (0, 2, 1, 3))


def hier_moe(x, w_rg, b_rg, w_re, b_re, w_gate, w_up, w_down):
    b, s, d = x.shape
    xt = x.reshape(b * s, d)
    g_prob = jax.nn.softmax((xt @ w_rg + b_rg).astype(jnp.float32), axis=-1)
    g_w, g_idx = lax.top_k(g_prob, 1)
    e_logits = (xt @ w_re + b_re).astype(jnp.float32).reshape(-1, N_GROUPS, EXPERTS_PER_GROUP)
    e_logits = jnp.take_along_axis(e_logits, g_idx[:, :, None], axis=1)[:, 0]
    e_w, e_idx = lax.top_k(jax.nn.softmax(e_logits, axis=-1), TOP_K_INNER)
    e_w = e_w / jnp.sum(e_w, axis=-1, keepdims=True)
    w = g_w * e_w
    expert_id = g_idx * EXPERTS_PER_GROUP + e_idx
    combine = jnp.sum(jax.nn.one_hot(expert_id, N_EXPERTS, dtype=jnp.float32) * w[..., None], axis=1)
    acc = jnp.zeros_like(xt)
    for e in range(N_EXPERTS):
        y = (jax.nn.silu(xt @ w_gate[e]) * (xt @ w_up[e])) @ w_down[e]
        acc = acc + combine[:, e:e + 1].astype(xt.dtype) * y
    return acc.reshape(b, s, d)


def setup_inputs(seed: int = 0) -> dict:
    key = jax.random.key(seed)
    ks = jax.random.split(key, 24)
    L, D = DEPTH, D_MODEL
    f32 = jnp.float32

    def nrm(k, shape, scale):
        return jax.random.normal(k, shape, f32) * scale

    beta = DEEPNORM_BETA
    col_scale = jnp.concatenate([
        jnp.ones((2 * FOX_WIDTH,), f32), jnp.full((FOX_WIDTH,), beta, f32),
        jnp.ones((2 * DIFF_QK_WIDTH,), f32), jnp.full((DIFF_WIDTH,), beta, f32),
        jnp.ones((MEM_WIDTH + N_BRANCH * D + FOX_HEADS,), f32)])
    mem_scale = jnp.concatenate([jnp.ones((MEM_WIDTH,), f32), jnp.full((MEM_WIDTH,), beta, f32)])
    return {
        "x": nrm(ks[0], (BATCH, SEQ, D), 1.0),
        "mem": nrm(ks[1], (BATCH, N_MEM, D), 1.0),
        "w_in": nrm(ks[2], (L, D, IN_COLS), D ** -0.5) * col_scale,
        "b_forget": FORGET_BIAS_OFFSET + nrm(ks[3], (L, FOX_HEADS), 0.5),
        "b_gates": nrm(ks[4], (L, N_BRANCH * D), 0.02),
        "lambda_q1": nrm(ks[5], (L, DIFF_QK_DIM), 0.1),
        "lambda_k1": nrm(ks[6], (L, DIFF_QK_DIM), 0.1),
        "lambda_q2": nrm(ks[7], (L, DIFF_QK_DIM), 0.1),
        "lambda_k2": nrm(ks[8], (L, DIFF_QK_DIM), 0.1),
        "diff_subln_g": 1.0 + nrm(ks[9], (L, DIFF_V_DIM), 0.02),
        "w_mem_kv": nrm(ks[10], (L, D, 2 * MEM_WIDTH), D ** -0.5) * mem_scale,
        "w_branch_fox": nrm(ks[11], (L, FOX_WIDTH, D), beta * FOX_WIDTH ** -0.5),
        "w_branch_diff": nrm(ks[12], (L, DIFF_WIDTH, D), beta * DIFF_WIDTH ** -0.5),
        "w_branch_mem": nrm(ks[13], (L, MEM_WIDTH, D), beta * MEM_WIDTH ** -0.5),
        "w_out": nrm(ks[14], (L, D, D), beta * D ** -0.5),
        "ln1_g": 1.0 + nrm(ks[15], (L, D), 0.02),
        "ln1_b": nrm(ks[16], (L, D), 0.02),
        "w_router_group": nrm(ks[17], (L, D, N_GROUPS), D ** -0.5),
        "b_router_group": nrm(ks[18], (L, N_GROUPS), 0.01),
        "w_router_expert": nrm(ks[19], (L, D, N_EXPERTS), D ** -0.5),
        "b_router_expert": nrm(ks[20], (L, N_EXPERTS), 0.01),
        "w_expert_gate": nrm(ks[21], (L, N_EXPERTS, D, D_EXPERT), beta * D ** -0.5),
        "w_expert_up": nrm(ks[22], (L, N_EXPERTS, D, D_EXPERT), beta * D ** -0.5),
        "w_expert_down": nrm(ks[23], (L, N_EXPERTS, D_EXPERT, D), beta * D_EXPERT ** -0.5),
        "ln2_g": 1.0 + nrm(jax.random.fold_in(key, 101), (L, D), 0.02),
        "ln2_b": nrm(jax.random.fold_in(key, 102), (L, D), 0.02),
    }


def reference(x, mem, w_in, b_forget, b_gates, lambda_q1, lambda_k1, lambda_q2, lambda_k2,
              diff_subln_g, w_mem_kv, w_branch_fox, w_branch_diff, w_branch_mem, w_out,
              ln1_g, ln1_b, w_router_group, b_router_group, w_router_expert, b_router_expert,
              w_expert_gate, w_expert_up, w_expert_down, ln2_g, ln2_b):
    B, S, D = x.shape
    slopes = 2.0 ** (-8.0 * jnp.arange(1, DIFF_HEADS + 1, dtype=jnp.float32) / DIFF_HEADS)
    for l in range(DEPTH):
        proj = x @ w_in[l]
        fq, fk, fv, dq, dk, dv, mq, gl, fl = jnp.split(proj, IN_OFFSETS, axis=-1)

        heads = lambda t, h, d: jnp.transpose(t.reshape(B, S, h, d), (0, 2, 1, 3))
        log_f = jax.nn.log_sigmoid((fl + b_forget[l]).astype(jnp.float32))
        y_fox = fox_attention(heads(fq, FOX_HEADS, HEAD_DIM), heads(fk, FOX_HEADS, HEAD_DIM),
                              heads(fv, FOX_HEADS, HEAD_DIM), jnp.transpose(log_f, (0, 2, 1)))
        y_fox = y_fox.reshape(B, S, FOX_WIDTH)

        lam_init = 0.8 - 0.6 * math.exp(-0.3 * l)
        lam = (jnp.exp(jnp.sum(lambda_q1[l] * lambda_k1[l]).astype(jnp.float32))
               - jnp.exp(jnp.sum(lambda_q2[l] * lambda_k2[l]).astype(jnp.float32)) + lam_init)
        dq = jnp.transpose(dq.reshape(B, S, DIFF_HEADS, 2, DIFF_QK_DIM), (0, 2, 1, 3, 4))
        dk = jnp.transpose(dk.reshape(B, S, DIFF_HEADS, 2, DIFF_QK_DIM), (0, 2, 1, 3, 4))
        y_diff = diff_attention(dq[..., 0, :], dq[..., 1, :], dk[..., 0, :], dk[..., 1, :],
                                heads(dv, DIFF_HEADS, DIFF_V_DIM), lam, slopes)
        y_diff = (rms_norm(y_diff, diff_subln_g[l]) * (1.0 - lam_init)).reshape(B, S, DIFF_WIDTH)

        mkv = mem @ w_mem_kv[l]
        mk, mv = jnp.split(mkv, 2, axis=-1)
        mem_heads = lambda t: jnp.transpose(t.reshape(B, N_MEM, MEM_HEADS, MEM_HEAD_DIM), (0, 2, 1, 3))
        y_mem = memory_attention(heads(mq, MEM_HEADS, MEM_HEAD_DIM), mem_heads(mk), mem_heads(mv))
        y_mem = y_mem.reshape(B, S, MEM_WIDTH)

        gates = jax.nn.sigmoid(gl + b_gates[l]).reshape(B, S, N_BRANCH, D)
        h = (gates[:, :, 0] * (y_fox @ w_branch_fox[l])
             + gates[:, :, 1] * (y_diff @ w_branch_diff[l])
             + gates[:, :, 2] * (y_mem @ w_branch_mem[l]))
        x = layer_norm(DEEPNORM_ALPHA * x + h @ w_out[l], ln1_g[l], ln1_b[l])

        m = hier_moe(x, w_router_group[l], b_router_group[l], w_router_expert[l], b_router_expert[l],
                     w_expert_gate[l], w_expert_up[l], w_expert_down[l])
        x = layer_norm(DEEPNORM_ALPHA * x + m, ln2_g[l], ln2_b[l])
    return x
```

```python
import os
from contextlib import ExitStack
import numpy as np
import concourse.bass as bass
import concourse.mybir as mybir
from concourse.bass_utils import run_bass_kernel_spmd

F32 = mybir.dt.float32
BF16 = mybir.dt.bfloat16
AF = mybir.ActivationFunctionType
ALU = mybir.AluOpType
AX = mybir.AxisListType

NCORES = 8
D = 1024
S = 4096
T = 2048
NE = 32
ALPHA = 2.0 ** 0.25
LN_EPS = 1e-5
OFF = dict(fq=0, fk=512, fv=1024, dq=1536, dk=2048, dv=2560, mq=3072, gl=3584, fl=6656)


class Prog:
    SEM_WRAP = int(os.environ.get("MK_WRAP", "1000"))

    def __init__(self, nc, stack, n_dma_sems=6):
        self.nc = nc
        self.stack = stack
        self.E = {"pe": nc.tensor, "act": nc.scalar, "dve": nc.vector,
                  "pool": nc.gpsimd, "sp": nc.sync}
        self.sems = {}
        self.cur = {}
        self.nsem = 0
        for e in self.E:
            self._new_sem(e)
        self.waited = {e: {} for e in self.E}
        self.bufs = {}
        self.dring = {}
        for q in ("sp", "pool", "act"):
            ring = []
            for i in range(n_dma_sems):
                k = f"d_{q}_{i}"
                h = stack.enter_context(nc.semaphore(k))
                self.sems[k] = h
                ring.append([h, k, 0])
            self.dring[q] = [ring, 0]
        self.ninst = {e: 0 for e in self.E}

    def _new_sem(self, e):
        self.nsem += 1
        k = f"s_{e}_{self.nsem}"
        h = self.stack.enter_context(self.nc.semaphore(k))
        self.cur[e] = [h, k, 0]
        self.sems[k] = h

    def _deps(self, reads, writes):
        deps = {}

        def add(d):
            for k, v in d.items():
                if deps.get(k, 0) < v:
                    deps[k] = v
        for key in reads:
            st = self.bufs.get(key)
            if st:
                add(st[0])
        for key in writes:
            st = self.bufs.get(key)
            if st:
                add(st[0])
                add(st[1])
        return deps

    def _wait(self, eng, deps, skip_self=False):
        e = self.E[eng]
        w = self.waited[eng]
        for k, v in deps.items():
            if skip_self and k.startswith(f"s_{eng}_"):
                continue
            if w.get(k, 0) < v:
                e.wait_ge(self.sems[k], v)
                w[k] = v

    def _mark(self, tok, reads, writes):
        k, v = tok
        for key in reads:
            st = self.bufs.setdefault(key, [{}, {}])
            if st[1].get(k, 0) < v:
                st[1][k] = v
        for key in writes:
            self.bufs[key] = [{k: v}, {}]

    def op(self, eng, fn, reads=(), writes=()):
        deps = self._deps(reads, writes)
        self._wait(eng, deps, skip_self=(eng == "pe"))
        c = self.cur[eng]
        if c[2] >= self.SEM_WRAP:
            self._new_sem(eng)
            c = self.cur[eng]
        ins = fn(self.E[eng])
        ins.then_inc(c[0], 1)
        c[2] += 1
        self.ninst[eng] += 1
        self._mark((c[1], c[2]), reads, writes)

    def dma(self, q, out, in_, reads=(), writes=(), **kw):
        deps = self._deps(reads, writes)
        ring, idx = self.dring[q]
        s = ring[idx % len(ring)]
        self.dring[q][1] = idx + 1
        if s[2] > 0:
            deps[s[1]] = max(deps.get(s[1], 0), s[2])
        self._wait(q, deps)
        ins = self.E[q].dma_start(out=out, in_=in_, **kw)
        s[2] += 16
        ins.then_inc(s[0], 16)
        self._mark((s[1], s[2]), reads, writes)

    def prewait(self, eng, rw_list):
        deps = {}
        for reads, writes in rw_list:
            for k, v in self._deps(reads, writes).items():
                if deps.get(k, 0) < v:
                    deps[k] = v
        self._wait(eng, deps, skip_self=(eng == "pe"))

    def barrier(self):
        toks = {}
        for e in self.E:
            c = self.cur[e]
            if c[2] > 0:
                toks[c[1]] = c[2]
        for q in self.dring:
            for s in self.dring[q][0]:
                if s[2] > 0:
                    toks[s[1]] = s[2]
        for e in self.E:
            self._wait(e, dict(toks), skip_self=True)
        self.bufs = {}

    def finish(self, keys, eng="sp"):
        deps = self._deps(keys, ())
        self._wait(eng, deps)


def attn_steps(G):
    steps = []
    for j in range(2 * G):
        for kb in (2 * j, 2 * j + 1, 16 + 2 * j, 16 + 2 * j + 1):
            steps.append((kb, 0, None))
    b = 4 * G
    steps += [(b, 0, "tri"), (16 + b, 0, "par1"), (b + 1, 128, "tri"), (16 + b + 1, 128, "par0"),
              (b + 2, 256, "tri"), (16 + b + 2, 256, "par1"), (b + 3, 384, "tri"), (16 + b + 3, 384, "par0")]
    return steps


def build_program(debug=False):
    nc = bass.Bass("TRN2", target_bir_lowering=False)

    def din(name, shape, dt=F32):
        return nc.dram_tensor(name, list(shape), dt, kind="ExternalInput").ap()

    xTo_d = din("xTo", [D, T]); xTr_d = din("xTr", [D, T]); xo_d = din("xo", [T, D])
    memT_d = din("memT", [D, 256])
    w_in = din("w_in", [D, 6664]); w_mkv = din("w_mkv", [D, 1024])
    w_br_d = [din("w_bf", [512, D]), din("w_bd", [512, D]), din("w_bm", [512, D])]
    w_out_d = din("w_out", [D, D]); w_r_d = din("w_r", [D, 36])
    w_eg = din("w_eg", [NE, D, 256]); w_eu = din("w_eu", [NE, D, 256]); w_ed = din("w_ed", [NE, 256, D])
    nbf_d = din("nbf", [8, 1]); bgt_d = din("bgt", [128, 24]); lam_d = din("lam4", [4, 64])
    sg_d = din("subln_g", [128, 1])
    ln_d = [din("ln1_g", [D]), din("ln1_b", [D]), din("ln2_g", [D]), din("ln2_b", [D])]
    b_r_d = din("b_r", [36])
    par_d = din("par", [128, 2]); dqa_d = din("dqaug", [4, 4, T]); dka_d = din("dkaug", [4, 4, S])
    rst_d = din("rst", [8, S])
    out_d = nc.dram_tensor("out", [T, D], F32, kind="ExternalOutput").ap()
    scr_k = nc.dram_tensor("scr_k", [8, 3, S], BF16, kind="Internal").ap()
    scr_q = nc.dram_tensor("scr_q", [8, 3, T], BF16, kind="Internal").ap()
    dbg = {}
    if debug:
        dbg["yT"] = nc.dram_tensor("dbg_yT", [128, 12, T], BF16, kind="ExternalOutput").ap()
        dbg["x1"] = nc.dram_tensor("dbg_x1", [T, D], F32, kind="ExternalOutput").ap()
        dbg["comb"] = nc.dram_tensor("dbg_comb", [128, 16, NE], F32, kind="ExternalOutput").ap()

    STOP = int(os.environ.get("MK_STOP", "99"))

    class _Stop(Exception):
        pass

    def ckpt(n):
        if STOP == n:
            P.barrier()
            raise _Stop()

    try:
      with ExitStack() as top:
          P = Prog(nc, top)

          def sbt(st, name, shape, dt=F32):
              return st.enter_context(nc.sbuf_tensor("sb_" + name, list(shape), dt))

          def mm(out, lhsT, rhs, start, stop, reads, writes):
              P.op("pe", lambda e: e.matmul(out, lhsT=lhsT, rhs=rhs, start=start, stop=stop), reads, writes)

          def tr(out, in_, reads, writes):
              P.op("pe", lambda e: e.transpose(out=out, in_=in_, identity=ident[:]), list(reads) + ["ident"], writes)

          def act(out, in_, func, reads, writes, bias=None, scale=None, accum_out=None):
              kw = {}
              if bias is not None:
                  kw["bias"] = bias
              if scale is not None:
                  kw["scale"] = scale
              if accum_out is not None:
                  kw["accum_out"] = accum_out
              P.op("act", lambda e: e.activation(out=out, in_=in_, func=func, **kw), reads, writes)

          def cp(eng, out, in_, reads, writes):
              if eng == "act":
                  P.op("act", lambda e: e.activation(out=out, in_=in_, func=AF.Copy), reads, writes)
              else:
                  P.op(eng, lambda e: e.tensor_copy(out=out, in_=in_), reads, writes)

          def tt(eng, out, in0, in1, op, reads, writes):
              P.op(eng, lambda e: e.tensor_tensor(out=out, in0=in0, in1=in1, op=op), reads, writes)

          def ts(eng, out, in0, s1, op0, reads, writes, s2=None, op1=None):
              if op1 is None:
                  P.op(eng, lambda e: e.tensor_scalar(out=out, in0=in0, scalar1=s1, scalar2=None, op0=op0), reads, writes)
              else:
                  P.op(eng, lambda e: e.tensor_scalar(out=out, in0=in0, scalar1=s1, scalar2=s2, op0=op0, op1=op1), reads, writes)

          def stt(eng, out, in0, scalar, in1, op0, op1, reads, writes):
              P.op(eng, lambda e: e.scalar_tensor_tensor(out=out, in0=in0, scalar=scalar, in1=in1, op0=op0, op1=op1), reads, writes)

          def memset(eng, ap, val, writes):
              P.op(eng, lambda e: e.memset(ap, val), (), writes)

          ps = [top.enter_context(nc.psum_tensor(f"ps{i}", [128, 512], F32)) for i in range(8)]
          PSK = [("ps", i) for i in range(8)]
          bufA = sbt(top, "bufA", [128, 8, T], BF16)
          bufB = sbt(top, "bufB", [128, 8, T], BF16)
          bufC = sbt(top, "bufC", [128, 16384], F32)
          bufCb = bufC[:].bitcast(BF16)
          yT = bufCb[:, 0:24576].rearrange("p (c t) -> p c t", c=12)
          KT = bufCb[:, 24576:32768].rearrange("p (h t) -> p h t", h=2)
          acc = bufC[:].rearrange("p (b d) -> p b d", b=16)
          xTo = bufA; xTr = bufB; x1T = bufA; hT = bufB
          ident = sbt(top, "ident", [128, 128]); onesb = sbt(top, "onesb", [128, 128], BF16)
          onesf = sbt(top, "onesf", [128, 64]); par = sbt(top, "par_sb", [128, 2])
          comb = sbt(top, "comb", [128, 16, NE])
          nlam = sbt(top, "nlam", [128, 1]); gs = sbt(top, "gs", [128, 1])

          memset("pool", ident[:], 1.0, ["ident"])
          P.op("pool", lambda e: e.affine_select(out=ident[:], in_=ident[:], pattern=[[-1, 128]], compare_op=ALU.is_equal,
                                                 fill=0.0, base=0, channel_multiplier=1), ["ident"], ["ident"])
          memset("pool", onesb[:], 1.0, ["onesb"])
          identb = sbt(top, "identb", [128, 128], BF16); Ttri = sbt(top, "Ttri", [128, 128], BF16)
          Zp = [sbt(top, f"Zp{i}", [128, 128], BF16) for i in range(2)]
          cp("pool", identb[:], ident[:], ["ident"], ["identb"])
          memset("pool", Ttri[:], 0.0, ["Ttri"])
          P.op("pool", lambda e: e.affine_select(out=Ttri[:], in_=Ttri[:], pattern=[[1, 128]], compare_op=ALU.is_ge,
                                                 fill=-30000.0, base=0, channel_multiplier=-1), ["Ttri"], ["Ttri"])
          memset("pool", onesf[:], 1.0, ["onesf"])
          P.dma("sp", par[:], par_d, writes=["par"])
          for i_ in range(2):
              ts("dve", Zp[i_][:], onesb[:], par[:, i_:i_ + 1], ALU.mult, ["onesb", "par"], [("Zp", i_)], s2=-1.0, op1=ALU.add)
              ts("dve", Zp[i_][:], Zp[i_][:], 30000.0, ALU.mult, [("Zp", i_)], [("Zp", i_)])

          for dc in range(8):
              P.dma("pool", xTo[:, dc, :], xTo_d[dc * 128:(dc + 1) * 128, :], writes=[("xTo", dc)])
          for dc in range(8):
              P.dma("pool", xTr[:, dc, :], xTr_d[dc * 128:(dc + 1) * 128, :], writes=[("xTr", dc)])
          XTO = [("xTo", dc) for dc in range(8)]
          XTR = [("xTr", dc) for dc in range(8)]
          ckpt(0)

          with ExitStack() as s1:
              pk = sbt(s1, "pk", [8, 3, S], BF16); pq = sbt(s1, "pq", [8, 3, T], BF16)
              wflf = sbt(s1, "wflf", [128, 8, 8]); wfl = sbt(s1, "wfl", [128, 8, 8], BF16)
              nbf = sbt(s1, "nbf_sb", [8, 1]); tot = sbt(s1, "tot", [8, 32]); base = sbt(s1, "base", [8, 32])
              sm = sbt(s1, "sm", [8, 4, 8])
              lamt = sbt(s1, "lamt", [128, 4, 64]); lame = sbt(s1, "lame", [128, 2]); lamp = sbt(s1, "lamp", [128, 2, 64])
              sgt = sbt(s1, "sgt", [128, 1])
              spv = bufC[0:8, 0:4096]; wv_ = bufC[0:8, 4096:8192]; rstv = bufC[0:8, 8192:12288]
              P.dma("sp", wflf[:], w_in[:, OFF["fl"]:OFF["fl"] + 8].rearrange("(c p) n -> p c n", p=128), writes=["wflf"])
              P.dma("sp", nbf[:], nbf_d, writes=["nbf"])
              P.dma("sp", rstv, rst_d, writes=["rst"])
              cp("dve", wfl[:], wflf[:], ["wflf"], ["wfl"])
              for i in range(4):
                  P.dma("sp", lamt[:, i, :], lam_d[i].partition_broadcast(128), writes=[("lamt", i)])
              P.dma("sp", sgt[:], sg_d, writes=["sgt"])
              tt("dve", lamp[:, 0, :], lamt[:, 0, :], lamt[:, 1, :], ALU.mult, [("lamt", 0), ("lamt", 1)], ["lamp0"])
              tt("dve", lamp[:, 1, :], lamt[:, 2, :], lamt[:, 3, :], ALU.mult, [("lamt", 2), ("lamt", 3)], ["lamp1"])
              P.op("dve", lambda e: e.reduce_sum(out=lame[:, 0:1], in_=lamp[:, 0, :], axis=AX.X), ["lamp0"], ["lame0"])
              P.op("dve", lambda e: e.reduce_sum(out=lame[:, 1:2], in_=lamp[:, 1, :], axis=AX.X), ["lamp1"], ["lame1"])
              act(lame[:], lame[:], AF.Exp, ["lame0", "lame1"], ["lame"])
              tt("dve", nlam[:], lame[:, 1:2], lame[:, 0:1], ALU.subtract, ["lame"], ["nlam"])
              ts("dve", nlam[:], nlam[:], -0.2, ALU.add, ["nlam"], ["nlam"])
              ts("dve", gs[:], sgt[:], 0.8, ALU.mult, ["sgt"], ["gs"])

              for g in range(8):
                  src, keys = (xTo, XTO) if g < 4 else (xTr, XTR)
                  bank = g % 2
                  for dc in range(8):
                      mm(ps[bank][0:8, :], wfl[:, dc, :], src[:, dc, (g % 4) * 512:(g % 4 + 1) * 512], dc == 0, dc == 7,
                         ["wfl", keys[dc]], [PSK[bank]])
                  sl = spv[:, g * 512:(g + 1) * 512]
                  act(sl, ps[bank][0:8, :], AF.Exp, [PSK[bank], "nbf"], [("sp", g)], bias=nbf[:], scale=-1.0)
                  act(sl, sl, AF.Ln, [("sp", g)], [("sp", g)], bias=1.0)
              SPK = [("sp", g) for g in range(8)]
              P.op("dve", lambda e: e.tensor_tensor_scan(out=wv_, data0=rstv, data1=spv, initial=0.0, op0=ALU.mult, op1=ALU.add),
                   SPK + ["rst"], ["w"])
              cp("dve", tot[:], wv_.rearrange("p (b i) -> p b i", i=128)[:, :, 127], ["w"], ["tot"])
              tv = tot[:].rearrange("p (s j t) -> p s j t", s=2, t=2)
              bv = base[:].rearrange("p (s j t) -> p s j t", s=2, t=2)
              tA, tB, tC, tD = tv[:, 0, :, 0], tv[:, 0, :, 1], tv[:, 1, :, 0], tv[:, 1, :, 1]
              Tj, AC, incl, Base = sm[:, 0, :], sm[:, 1, :], sm[:, 2, :], sm[:, 3, :]
              tt("dve", Tj, tA, tB, ALU.add, ["tot"], ["Tj"])
              tt("dve", AC, tC, tD, ALU.add, ["tot"], ["AC"])
              tt("dve", Tj, Tj, AC, ALU.add, ["Tj", "AC"], ["Tj"])
              P.op("dve", lambda e: e.tensor_tensor_scan(out=incl, data0=onesf[0:8, 0:8], data1=Tj, initial=0.0, op0=ALU.mult, op1=ALU.add),
                   ["Tj", "onesf"], ["incl"])
              tt("dve", Base, incl, Tj, ALU.subtract, ["incl", "Tj"], ["Base"])
              p0, p1 = par[0:8, 0:1], par[0:8, 1:2]
              stt("dve", bv[:, 0, :, 0], tC, p1, Base, ALU.mult, ALU.add, ["tot", "Base", "par"], ["bA"])
              stt("dve", bv[:, 1, :, 0], tA, p0, Base, ALU.mult, ALU.add, ["tot", "Base", "par"], ["bC"])
              tt("dve", AC, tA, tC, ALU.add, ["tot", "AC"], ["AC"])
              tt("dve", AC, AC, Base, ALU.add, ["AC", "Base"], ["AC"])
              stt("dve", bv[:, 0, :, 1], tD, p0, AC, ALU.mult, ALU.add, ["tot", "AC", "par"], ["bB"])
              stt("dve", bv[:, 1, :, 1], tB, p1, AC, ALU.mult, ALU.add, ["tot", "AC", "par"], ["bD"])
              for blk in range(32):
                  sl = wv_[:, blk * 128:(blk + 1) * 128]
                  ts("dve", sl, sl, base[:, blk:blk + 1], ALU.add, ["w", "bA", "bB", "bC", "bD"], ["w"])
              cp("dve", pk[:, 0, :], wv_, ["w"], ["pk0"])
              tt("dve", spv, wv_, pk[:, 0, :], ALU.subtract, ["w", "pk0"] + SPK, ["r"])
              cp("dve", pk[:, 1, :], spv, ["r"], ["pk1"])
              tt("dve", spv, spv, pk[:, 1, :], ALU.subtract, ["r", "pk1"], ["r"])
              cp("dve", pk[:, 2, :], spv, ["r"], ["pk2"])
              ts("dve", pq[:], pk[:, :, 0:T], -1.0, ALU.mult, ["pk0", "pk1", "pk2"], ["pq"])
              P.dma("sp", scr_k, pk[:], reads=["pk0", "pk1", "pk2"], writes=["scr_k"])
              P.dma("sp", scr_q, pq[:], reads=["pq"], writes=["scr_q"])
              P.barrier()
              ckpt(1)
          P.bufs["scr_k"] = [{}, {}]

          with ExitStack() as s2:
              QT = sbt(s2, "QT", [128, 2, T], BF16)
              Vb = sbt(s2, "Vb", [128, 32, 132], BF16)
              PT = [sbt(s2, f"PT{i}", [128, 512], BF16) for i in range(4)]
              wsl = [[sbt(s2, f"wsl{i}{j}", [128, 8, 128], BF16) for j in range(3)] for i in range(2)]
              stg = [sbt(s2, f"stg{i}", [128, 8, 128]) for i in range(3)]
              denrow = [sbt(s2, f"denrow{i}", [128, 512]) for i in range(2)]
              rec = [sbt(s2, f"rec{i}", [128, 512]) for i in range(2)]
              t1 = sbt(s2, "t1", [128, 512]); t2 = sbt(s2, "t2", [128, 512]); sqb = sbt(s2, "sqb", [128, 512], BF16)
              stgc = [0]

              def load_w(dst, dkey, col0):
                  i = stgc[0] % 3
                  stgc[0] += 1
                  P.dma("sp", stg[i][:], w_in[:, col0:col0 + 128].rearrange("(c p) n -> p c n", p=128), writes=[("stg", i)])
                  cp("pool", dst[:], stg[i][:], [("stg", i)], [dkey])

              def proj_KQ(slot, bank0, qscale_eng="act"):
                  wk, wq = wsl[slot][0], wsl[slot][1]
                  bi = 0
                  for g in range(8):
                      src, keys = (xTo, XTO) if g < 4 else (xTr, XTR)
                      bank = bank0 + (bi % 2); bi += 1
                      for dc in range(8):
                          mm(ps[bank][:, :], wk[:, dc, :], src[:, dc, (g % 4) * 512:(g % 4 + 1) * 512], dc == 0, dc == 7,
                             [("wsl", slot, 0), keys[dc]], [PSK[bank]])
                      cp("dve", KT[0:64, 0, g * 512:(g + 1) * 512], ps[bank][0:64, :], [PSK[bank]], [("KT", 0, g)])
                      cp("act", KT[0:64, 1, g * 512:(g + 1) * 512], ps[bank][64:128, :], [PSK[bank]], [("KT", 1, g)])
                  for g in range(4):
                      bank = bank0 + (bi % 2); bi += 1
                      for dc in range(8):
                          mm(ps[bank][:, :], wq[:, dc, :], xTo[:, dc, g * 512:(g + 1) * 512], dc == 0, dc == 7,
                             [("wsl", slot, 1), XTO[dc]], [PSK[bank]])
                      ts("dve", QT[0:64, 0, g * 512:(g + 1) * 512], ps[bank][0:64, :], 0.125, ALU.mult, [PSK[bank]], [("QT", 0, g)])
                      act(QT[0:64, 1, g * 512:(g + 1) * 512], ps[bank][64:128, :], AF.Copy, [PSK[bank]], [("QT", 1, g)], scale=0.125)

              def proj_V(slot, bank0, fox):
                  wvv = wsl[slot][2]
                  for b4 in range(8):
                      bank = bank0 + (b4 % 2)
                      for bb in range(4):
                          blk = b4 * 4 + bb
                          src, keys = (xTo, XTO) if blk < 16 else (xTr, XTR)
                          for dc in range(8):
                              mm(ps[bank][:, bb * 128:(bb + 1) * 128], src[:, dc, (blk % 16) * 128:(blk % 16 + 1) * 128], wvv[:, dc, :],
                                 dc == 0, dc == 7, [("wsl", slot, 2), keys[dc]], [PSK[bank]])
                      pv = ps[bank][:, :].rearrange("p (b c) -> p b c", b=4)
                      if fox:
                          cp("dve", Vb[:, b4 * 4:(b4 + 1) * 4, 0:64], pv[:, :, 0:64], [PSK[bank]], [("V", 0, b4)])
                          cp("dve", Vb[:, b4 * 4:(b4 + 1) * 4, 66:130], pv[:, :, 64:128], [PSK[bank]], [("V", 1, b4)])
                      else:
                          eng = "dve" if b4 % 2 == 0 else "act"
                          cp(eng, Vb[:, b4 * 4:(b4 + 1) * 4, 0:128], pv, [PSK[bank]], [("V", 0, b4), ("V", 1, b4)])

              def maskmm(bank, kind):
                  M, mk = {"tri": (Ttri, "Ttri"), "par0": (Zp[0], ("Zp", 0)), "par1": (Zp[1], ("Zp", 1))}[kind]
                  mm(ps[bank][:, 0:128], identb[:, :], M[:, :], False, True, ["identb", mk], [PSK[bank]])

              KTk = lambda hh, kb: [("KT", hh, kb // 4), ("KTa", hh)]
              QTk = lambda hh, G: [("QT", hh, G), ("QTa", hh)]

              memset("dve", KT[64:70, :, :], 1.0, [("KTa", 0), ("KTa", 1)])
              memset("dve", QT[64:70, :, :], 1.0, [("QTa", 0), ("QTa", 1)])
              memset("pool", Vb[:, :, 64:65], 1.0, [("Vone", 0)])
              memset("pool", Vb[:, :, 130:131], 1.0, [("Vone", 1)])
              for j, nm in enumerate(("fk", "fq", "fv")):
                  load_w(wsl[0][j], ("wsl", 0, j), OFF[nm])
              sctr = [0]; pctr = [0]; actr = [0]; pend = []
              for hp in range(4):
                  slot = hp % 2
                  if hp + 1 < 4:
                      for j, nm in enumerate(("fk", "fq", "fv")):
                          load_w(wsl[1 - slot][j], ("wsl", 1 - slot, j), OFF[nm] + (hp + 1) * 128)
                  ckpt(20)
                  proj_KQ(slot, 6)
                  ckpt(21)
                  proj_V(slot, 6, True)
                  ckpt(22)
                  for hh in range(2):
                      h = 2 * hp + hh
                      P.dma("sp", KT[67:70, hh, :], scr_k[h], reads=["scr_k"], writes=[("KTa", hh)])
                      P.dma("sp", QT[64:67, hh, :], scr_q[h], reads=["scr_q"], writes=[("QTa", hh)])
                  ckpt(23)
                  for hh in range(2):
                      for G in range(4):
                          if G == 1:
                              ckpt(25)
                          steps = attn_steps(G)
                          n = len(steps)
                          ab = 4 + (actr[0] % 2); actr[0] += 1
                          chunks = [steps[k:k + 2] for k in range(0, n, 2)]
                          info = {}
                          for ci in range(len(chunks) + 1):
                              if ci < len(chunks):
                                  rw = []
                                  cur = []
                                  for (kb, c0, spc) in chunks[ci]:
                                      sb_ = sctr[0] % 4; sctr[0] += 1
                                      pb = pctr[0] % 4; pctr[0] += 1
                                      cur.append((kb, c0, spc, sb_, pb))
                                      rw.append((KTk(hh, kb) + QTk(hh, G) + ["identb", "Ttri", ("Zp", 0), ("Zp", 1)], [PSK[sb_]]))
                                  info[ci] = cur
                                  P.prewait("pe", rw)
                                  for (kb, c0, spc, sb_, pb) in cur:
                                      w = 512 - c0
                                      mm(ps[sb_][:, 0:w], KT[0:70, hh, kb * 128:(kb + 1) * 128], QT[0:70, hh, G * 512 + c0:(G + 1) * 512],
                                         True, spc is None, KTk(hh, kb) + QTk(hh, G), [PSK[sb_]])
                                      if spc:
                                          maskmm(sb_, spc)
                                  for (kb, c0, spc, sb_, pb) in cur:
                                      w = 512 - c0
                                      act(PT[pb][:, 0:w], ps[sb_][:, 0:w], AF.Exp, [PSK[sb_]], [("PT", pb)])
                              if ci == 2 and pend:
                                  pend.pop(0)()
                              if ci - 1 >= 0:
                                  cur = info[ci - 1]
                                  P.prewait("pe", [([("V", hh, kb // 4), ("Vone", hh), ("PT", pb)], [PSK[ab]]) for (kb, c0, spc, sb_, pb) in cur])
                                  for j_, (kb, c0, spc, sb_, pb) in enumerate(cur):
                                      w = 512 - c0
                                      ii = 2 * (ci - 1) + j_
                                      mm(ps[ab][0:65, c0:512], Vb[:, kb, hh * 66:hh * 66 + 65], PT[pb][:, 0:w], ii == 0, ii == n - 1,
                                         [("V", hh, kb // 4), ("Vone", hh), ("PT", pb)], [PSK[ab]])
                          ckpt(24)
                          d = ab - 4
                          cp("dve", denrow[d][64:65, :], ps[ab][64:65, :], [PSK[ab]], [("denrow", d)])

                          def fin(d=d, ab=ab, hh=hh, hp=hp, G=G):
                              mm(ps[6][0:64, :], onesf[64:65, 0:64], denrow[d][64:65, :], True, True, [("denrow", d), "onesf"], [PSK[6]])
                              P.op("dve", lambda e: e.reciprocal(out=rec[d][0:64, :], in_=ps[6][0:64, :]), [PSK[6]], [("rec", d)])
                              tt("dve", yT[hh * 64:(hh + 1) * 64, hp, G * 512:(G + 1) * 512], ps[ab][0:64, :], rec[d][0:64, :], ALU.mult,
                                 [PSK[ab], ("rec", d)], [("yT", hp, G, hh)])
                          pend.append(fin)

              while pend:
                  pend.pop(0)()
              ckpt(2)
              for j, nm in enumerate(("dk", "dq", "dv")):
                  load_w(wsl[0][j], ("wsl", 0, j), OFF[nm])
              for h in range(4):
                  slot = h % 2
                  if h + 1 < 4:
                      for j, nm in enumerate(("dk", "dq", "dv")):
                          load_w(wsl[1 - slot][j], ("wsl", 1 - slot, j), OFF[nm] + (h + 1) * 128)
                  proj_KQ(slot, 0)
                  proj_V(slot, 2, False)
                  for hh in range(2):
                      for half in range(2):
                          P.dma("pool", KT[64:68, hh, half * 2048:(half + 1) * 2048], dka_d[h, :, half * 2048:(half + 1) * 2048],
                                writes=[("KTa", hh)] if half == 1 else [("KTa", hh)])
                      P.dma("pool", QT[64:68, hh, :], dqa_d[h], writes=[("QTa", hh)])
                  for G in range(4):
                      steps = attn_steps(G)
                      n = len(steps)
                      slots = {}
                      for i in range(n + 1):
                          if i < n:
                              kb, c0, spc = steps[i]
                              w = 512 - c0
                              sl_ = []
                              rw = []
                              for hh in range(2):
                                  sb_ = sctr[0] % 4; sctr[0] += 1
                                  pb = pctr[0] % 4; pctr[0] += 1
                                  sl_.append((sb_, pb))
                                  rw.append((KTk(hh, kb) + QTk(hh, G) + ["identb", "Ttri", ("Zp", 0), ("Zp", 1)], [PSK[sb_]]))
                              slots[i] = sl_
                              P.prewait("pe", rw)
                              for hh in range(2):
                                  sb_, pb = sl_[hh]
                                  mm(ps[sb_][:, 0:w], KT[0:68, hh, kb * 128:(kb + 1) * 128], QT[0:68, hh, G * 512 + c0:(G + 1) * 512],
                                     True, spc is None, KTk(hh, kb) + QTk(hh, G), [PSK[sb_]])
                                  if spc:
                                      maskmm(sb_, spc)
                              for hh in range(2):
                                  sb_, pb = sl_[hh]
                                  act(PT[pb][:, 0:w], ps[sb_][:, 0:w], AF.Exp, [PSK[sb_]], [("PT", pb)])
                          if i - 1 >= 0:
                              ii = i - 1
                              kb, c0, spc = steps[ii]
                              w = 512 - c0
                              VK = [("V", 0, kb // 4), ("V", 1, kb // 4), "onesb"]
                              P.prewait("pe", [(VK + [("PT", slots[ii][hh][1])], [PSK[4 + 2 * hh], PSK[5 + 2 * hh]]) for hh in range(2)])
                              for hh in range(2):
                                  _, pb = slots[ii][hh]
                                  mm(ps[4 + 2 * hh][:, c0:512], Vb[:, kb, 0:128], PT[pb][:, 0:w], ii == 0, ii == n - 1,
                                     [("V", 0, kb // 4), ("V", 1, kb // 4), ("PT", pb)], [PSK[4 + 2 * hh]])
                                  mm(ps[5 + 2 * hh][:, c0:512], onesb[:, :], PT[pb][:, 0:w], ii == 0, ii == n - 1,
                                     ["onesb", ("PT", pb)], [PSK[5 + 2 * hh]])
                      act(t1[:], ps[5][:, :], AF.Ln, [PSK[5]], ["t1"])
                      act(t2[:], ps[7][:, :], AF.Ln, [PSK[7]], ["t2"])
                      act(t1[:], t1[:], AF.Exp, ["t1"], ["t1"], scale=-1.0)
                      act(t2[:], t2[:], AF.Exp, ["t2"], ["t2"], scale=-1.0)
                      tt("dve", t1[:], ps[4][:, :], t1[:], ALU.mult, [PSK[4], "t1"], ["t1"])
                      tt("dve", t2[:], ps[6][:, :], t2[:], ALU.mult, [PSK[6], "t2"], ["t2"])
                      stt("dve", t1[:], t2[:], nlam[:], t1[:], ALU.mult, ALU.add, ["t1", "t2", "nlam"], ["t1"])
                      tt("dve", sqb[:], t1[:], t1[:], ALU.mult, ["t1"], ["sqb"])
                      mb = sctr[0] % 4; sctr[0] += 1
                      mm(ps[mb][:, :], onesb[:, :], sqb[:], True, True, ["onesb", "sqb"], [PSK[mb]])
                      act(t2[:], ps[mb][:, :], AF.Ln, [PSK[mb]], ["t2"], bias=LN_EPS, scale=1.0 / 128.0)
                      act(t2[:], t2[:], AF.Exp, ["t2"], ["t2"], scale=-0.5)
                      tt("dve", t1[:], t1[:], t2[:], ALU.mult, ["t1", "t2"], ["t1"])
                      ts("dve", yT[:, 4 + h, G * 512:(G + 1) * 512], t1[:], gs[:], ALU.mult, ["t1", "gs"], [("yT", 4 + h, G)])
              P.barrier()
              ckpt(3)

          with ExitStack() as s3:
              memTs = sbt(s3, "memTs", [128, 8, 256], BF16)
              wm = [sbt(s3, f"wm{i}", [128, 8, 512], BF16) for i in range(3)]
              stg8 = [sbt(s3, f"stg8_{i}", [128, 8, 256]) for i in range(2)]
              mkT = sbt(s3, "mkT", [128, 4, 256], BF16); mv = sbt(s3, "mv", [128, 2, 512], BF16)
              mqs = [sbt(s3, f"mqs{i}", [128, 512], BF16) for i in range(2)]
              PTm = [sbt(s3, f"PTm{i}", [128, 512], BF16) for i in range(4)]
              recm = sbt(s3, "recm", [128, 512])
              for dc in range(8):
                  P.dma("pool", memTs[:, dc, :], memT_d[dc * 128:(dc + 1) * 128, :], writes=[("memT", dc)])
              MK = [("memT", dc) for dc in range(8)]
              k8 = 0
              for i, (srcw, c0) in enumerate(((w_mkv, 0), (w_mkv, 512), (w_in, OFF["mq"]))):
                  for hf in range(2):
                      si = k8 % 2; k8 += 1
                      P.dma("sp", stg8[si][:], srcw[:, c0 + hf * 256:c0 + (hf + 1) * 256].rearrange("(c p) n -> p c n", p=128),
                            writes=[("stg8", si)])
                      cp("pool", wm[i][:, :, hf * 256:(hf + 1) * 256], stg8[si][:], [("stg8", si)], [("wm", i, hf)])
              WM = lambda i: [("wm", i, 0), ("wm", i, 1)]
              for h in range(4):
                  for dc in range(8):
                      mm(ps[0][:, 0:256], wm[0][:, dc, h * 128:(h + 1) * 128], memTs[:, dc, :], dc == 0, dc == 7, WM(0) + [MK[dc]], [PSK[0]])
                  cp("dve", mkT[:, h, :], ps[0][:, 0:256], [PSK[0]], [("mkT", h)])
              for mb in range(2):
                  for dc in range(8):
                      mm(ps[1][:, :], memTs[:, dc, mb * 128:(mb + 1) * 128], wm[1][:, dc, :], dc == 0, dc == 7, WM(1) + [MK[dc]], [PSK[1]])
                  cp("dve", mv[:, mb, :], ps[1][:, :], [PSK[1]], [("mv", mb)])
              def stM1(n_):
                  h, G, qb = n_ // 4, n_ % 4, n_ % 2
                  bq = 0 + qb
                  for dc in range(8):
                      mm(ps[bq][:, :], wm[2][:, dc, h * 128:(h + 1) * 128], xTo[:, dc, G * 512:(G + 1) * 512], dc == 0, dc == 7,
                         WM(2) + [XTO[dc]], [PSK[bq]])
                  act(mqs[qb][:], ps[bq][:, :], AF.Copy, [PSK[bq]], [("mqs", qb)], scale=128.0 ** -0.5)

              def stM2(n_):
                  h, G, qb = n_ // 4, n_ % 4, n_ % 2
                  bn_, bd_ = 4 + 2 * qb, 5 + 2 * qb
                  for mb in range(2):
                      sbk = 2 + mb
                      pb = (2 * qb + mb)
                      mm(ps[sbk][:, :], mkT[:, h, mb * 128:(mb + 1) * 128], mqs[qb][:], True, True, [("mkT", h), ("mqs", qb)], [PSK[sbk]])
                      act(PTm[pb][:], ps[sbk][:, :], AF.Exp, [PSK[sbk]], [("PTm", pb)])
                  for mb in range(2):
                      pb = (2 * qb + mb)
                      mm(ps[bn_][:, :], mv[:, mb, h * 128:(h + 1) * 128], PTm[pb][:], mb == 0, mb == 1, [("mv", mb), ("PTm", pb)], [PSK[bn_]])
                      mm(ps[bd_][:, :], onesb[:, :], PTm[pb][:], mb == 0, mb == 1, ["onesb", ("PTm", pb)], [PSK[bd_]])
                  P.op("dve", lambda e: e.reciprocal(out=recm[:], in_=ps[bd_][:, :]), [PSK[bd_]], ["recm"])
                  tt("dve", yT[:, 8 + h, G * 512:(G + 1) * 512], ps[bn_][:, :], recm[:], ALU.mult, [PSK[bn_], "recm"], [("yT", 8 + h, G)])

              stM1(0)
              for n_ in range(16):
                  if n_ + 1 < 16:
                      stM1(n_ + 1)
                  stM2(n_)
              if debug:
                  P.dma("sp", dbg["yT"], yT, reads=[k for k in P.bufs if isinstance(k, tuple) and k[0] == "yT"], writes=["dbg_yT"])
              P.barrier()
              ckpt(4)

          with ExitStack() as s4:
              wgl = [[sbt(s4, f"wgl{i}{b}", [128, 8, 128], BF16) for b in range(3)] for i in range(2)]
              wbr = [[sbt(s4, f"wbr{i}{b}", [128, 4, 128], BF16) for b in range(3)] for i in range(2)]
              stgA = [sbt(s4, f"stgA{i}", [128, 8, 128]) for i in range(3)]
              bgt = sbt(s4, "bgt", [128, 24])
              gsb = [sbt(s4, f"gsb{i}", [128, 512]) for i in range(2)]
              hac = [sbt(s4, f"hac{i}", [128, 512]) for i in range(2)]
              P.dma("sp", bgt[:], bgt_d, writes=["bgt"])
              sa = [0]

              def load_fc(slot, fc):
                  for br in range(3):
                      i = sa[0] % 3; sa[0] += 1
                      c0 = OFF["gl"] + br * 1024 + fc * 128
                      P.dma("sp", stgA[i][:], w_in[:, c0:c0 + 128].rearrange("(c p) n -> p c n", p=128), writes=[("stgA", i)])
                      cp("pool", wgl[slot][br][:], stgA[i][:], [("stgA", i)], [("wgl", slot, br)])
                      i = sa[0] % 3; sa[0] += 1
                      P.dma("sp", stgA[i][:, 0:4, :], w_br_d[br][:, fc * 128:(fc + 1) * 128].rearrange("(c p) n -> p c n", p=128),
                            writes=[("stgA", i)])
                      cp("pool", wbr[slot][br][:], stgA[i][:, 0:4, :], [("stgA", i)], [("wbr", slot, br)])

              load_fc(0, 0)
              it = 0
              for fc in range(8):
                  slot = fc % 2
                  if fc + 1 < 8:
                      load_fc(1 - slot, fc + 1)
                  for G in range(4):
                      hb = it % 2; it += 1
                      for br in range(3):
                          zb = (br % 2); gb = 2 + (br % 2)
                          if br == 2:
                              zb, gb = 4, 5
                          for kc in range(4):
                              mm(ps[zb][:, :], wbr[slot][br][:, kc, :], yT[:, br * 4 + kc, G * 512:(G + 1) * 512], kc == 0, kc == 3,
                                 [("wbr", slot, br)], [PSK[zb]])
                          for dc in range(8):
                              mm(ps[gb][:, :], wgl[slot][br][:, dc, :], xTo[:, dc, G * 512:(G + 1) * 512], dc == 0, dc == 7,
                                 [("wgl", slot, br)], [PSK[gb]])
                          gt = gsb[br % 2]
                          act(gt[:], ps[gb][:, :], AF.Sigmoid, [PSK[gb], "bgt"], [("gsb", br % 2)], bias=bgt[:, br * 8 + fc:br * 8 + fc + 1])
                          if br == 0:
                              tt("dve", hac[hb][:], gt[:], ps[zb][:, :], ALU.mult, [("gsb", 0), PSK[zb]], [("hac", hb)])
                          else:
                              tt("dve", gt[:], gt[:], ps[zb][:, :], ALU.mult, [("gsb", br % 2), PSK[zb]], [("gsb", br % 2)])
                              if br == 1:
                                  tt("dve", hac[hb][:], hac[hb][:], gt[:], ALU.add, [("hac", hb), ("gsb", 1)], [("hac", hb)])
                              else:
                                  tt("dve", hT[:, fc, G * 512:(G + 1) * 512], hac[hb][:], gt[:], ALU.add, [("hac", hb), ("gsb", 0)], [("hT", fc, G)])
              P.barrier()
              ckpt(5)

          with ExitStack() as s5:
              wo = sbt(s5, "wo", [128, 8, 1024], BF16)
              stgB = [sbt(s5, f"stgB{i}", [128, 2, 1024]) for i in range(2)]
              xres = [sbt(s5, f"xres{i}", [128, 1024]) for i in range(2)]
              rr = [sbt(s5, f"rr{i}", [128, 1024]) for i in range(2)]
              xf = [sbt(s5, f"xf{i}", [128, 8, 128]) for i in range(2)]
              lng = sbt(s5, "lng", [128, 1024]); lnb = sbt(s5, "lnb", [128, 1024])
              wr = sbt(s5, "wr", [128, 8, 36]); brt = sbt(s5, "brt", [128, 36])
              st_ = [sbt(s5, f"st{i}", [128, 2, 6]) for i in range(2)]
              mvv = [sbt(s5, f"mv{i}", [128, 2]) for i in range(2)]
              sml = [sbt(s5, f"sml{i}", [128, 16]) for i in range(2)]
              lg = [sbt(s5, f"lg{i}", [128, 36]) for i in range(2)]
              me = [sbt(s5, f"me{i}", [128, 32]) for i in range(2)]
              oh = [sbt(s5, f"oh{i}", [128, 2, 32]) for i in range(2)]
              top8 = [sbt(s5, f"top8{i}", [128, 8]) for i in range(2)]
              for c in range(4):
                  i = c % 2
                  P.dma("sp", stgB[i][:], w_out_d[c * 256:(c + 1) * 256, :].rearrange("(c p) n -> p c n", p=128), writes=[("stgB", i)])
                  cp("pool", wo[:, c * 2:(c + 1) * 2, :], stgB[i][:], [("stgB", i)], [("wo", c)])
              WO = [("wo", c) for c in range(4)]
              P.dma("sp", lng[:], ln_d[0].partition_broadcast(128), writes=["lng"])
              P.dma("sp", lnb[:], ln_d[1].partition_broadcast(128), writes=["lnb"])
              P.dma("sp", wr[:], w_r_d.rearrange("(c p) n -> p c n", p=128), writes=["wr"])
              P.dma("sp", brt[:], b_r_d.partition_broadcast(128), writes=["brt"])
              if True:
                  ckpt(60)
              def stA1(blk):
                  i = blk % 2
                  G = blk // 4
                  RR = [("rr", i, 0), ("rr", i, 1)]
                  LG = [("lg", i)]
                  s_ = sml[i]
                  SK = [("smr", i)]
                  P.dma("sp", xres[i][:], xo_d[blk * 128:(blk + 1) * 128, :], writes=[("xres", i)])
                  for ch in range(2):
                      bank = 2 * i + ch
                      for fc in range(8):
                          mm(ps[bank][:, :], hT[:, fc, blk * 128:(blk + 1) * 128], wo[:, fc, ch * 512:(ch + 1) * 512], fc == 0, fc == 7,
                             [("hT", fc, G)] + WO, [PSK[bank]])
                      stt("dve", rr[i][:, ch * 512:(ch + 1) * 512], xres[i][:, ch * 512:(ch + 1) * 512], ALPHA, ps[bank][:, :],
                          ALU.mult, ALU.add, [("xres", i), PSK[bank]], [("rr", i, ch)])
                      P.op("dve", lambda e: e.bn_stats(out=st_[i][:, ch, :], in_=rr[i][:, ch * 512:(ch + 1) * 512]), [("rr", i, ch)], [("st", i, ch)])
                  P.op("dve", lambda e: e.bn_aggr(out=mvv[i][:], in_=st_[i][:].rearrange("p a b -> p (a b)")), [("st", i, 0), ("st", i, 1)], [("mvv", i)])
                  act(sml[i][:, 0:1], mvv[i][:, 1:2], AF.Ln, [("mvv", i)], [("sml", i)], bias=LN_EPS)
                  act(sml[i][:, 0:1], sml[i][:, 0:1], AF.Exp, [("sml", i)], [("sml", i)], scale=-0.5)
                  RR = [("rr", i, 0), ("rr", i, 1)]
                  ts("dve", rr[i][:], rr[i][:], mvv[i][:, 0:1], ALU.subtract, RR + [("mvv", i), ("sml", i)], RR, s2=sml[i][:, 0:1], op1=ALU.mult)
                  tt("pool", rr[i][:], rr[i][:], lng[:], ALU.mult, RR + ["lng"], RR)
                  tt("pool", rr[i][:], rr[i][:], lnb[:], ALU.add, RR + ["lnb"], RR)
                  if debug:
                      P.dma("sp", dbg["x1"][blk * 128:(blk + 1) * 128, :], rr[i][:], reads=RR, writes=[("dbg_x1", blk)])
                  act(acc[:, blk, :], rr[i][:], AF.Copy, RR, [("acc", blk)], scale=ALPHA)

              def stA2(blk):
                  i = blk % 2
                  G = blk // 4
                  RR = [("rr", i, 0), ("rr", i, 1)]
                  LG = [("lg", i)]
                  s_ = sml[i]
                  SK = [("smr", i)]
                  for c in range(8):
                      bank = 4 + 2 * i + c // 4
                      tr(ps[bank][:, (c % 4) * 128:(c % 4 + 1) * 128], rr[i][:, c * 128:(c + 1) * 128], RR, [PSK[bank]])
                  for hf in range(2):
                      bank = 4 + 2 * i + hf
                      pv = ps[bank][:, :].rearrange("p (c t) -> p c t", c=4)
                      cp("dve", xf[i][:, hf * 4:(hf + 1) * 4, :], pv, [PSK[bank]], [("xf", i, hf)])
                      cp("act", x1T[:, hf * 4:(hf + 1) * 4, blk * 128:(blk + 1) * 128], xf[i][:, hf * 4:(hf + 1) * 4, :], [("xf", i, hf)], [("x1T", blk, hf)])
                  rb = 4 + 2 * i
                  for dc in range(8):
                      mm(ps[rb][:, 0:36], xf[i][:, dc, :], wr[:, dc, :], dc == 0, dc == 7, [("xf", i, 0), ("xf", i, 1), "wr"], [PSK[rb]])
                  LG = [("lg", i)]
                  tt("dve", lg[i][:], ps[rb][:, 0:36], brt[:], ALU.add, [PSK[rb], "brt"], LG)
                  s_ = sml[i]
                  SK = [("smr", i)]

              def stB(blk):
                  i = blk % 2
                  G = blk // 4
                  RR = [("rr", i, 0), ("rr", i, 1)]
                  LG = [("lg", i)]
                  s_ = sml[i]
                  SK = [("smr", i)]
                  tt("dve", s_[:, 12:13], lg[i][:, 0:1], lg[i][:, 1:2], ALU.max, LG, SK)
                  tt("dve", s_[:, 13:14], lg[i][:, 2:3], lg[i][:, 3:4], ALU.max, LG + SK, SK)
                  tt("dve", s_[:, 1:2], s_[:, 12:13], s_[:, 13:14], ALU.max, SK, SK)
                  ts("dve", s_[:, 2:3], s_[:, 1:2], -1.0, ALU.mult, SK, SK)
                  act(s_[:, 8:12], lg[i][:, 0:4], AF.Exp, LG + SK, SK, bias=s_[:, 2:3])
                  tt("dve", s_[:, 12:13], s_[:, 8:9], s_[:, 9:10], ALU.add, SK, SK)
                  tt("dve", s_[:, 13:14], s_[:, 10:11], s_[:, 11:12], ALU.add, SK, SK)
                  tt("dve", s_[:, 3:4], s_[:, 12:13], s_[:, 13:14], ALU.add, SK, SK)
                  P.op("dve", lambda e: e.reciprocal(out=s_[:, 4:5], in_=s_[:, 3:4]), SK, SK)
                  ts("dve", s_[:, 8:12], lg[i][:, 0:4], s_[:, 1:2], ALU.is_equal, LG + SK, SK)
                  ts("dve", s_[:, 8:12], s_[:, 8:12], 1.0e4, ALU.mult, SK, SK, s2=-1.0e4, op1=ALU.add)
                  for g in range(4):
                      ts("dve", me[i][:, g * 8:(g + 1) * 8], lg[i][:, 4 + g * 8:4 + (g + 1) * 8], s_[:, 8 + g:9 + g], ALU.add,
                         LG + SK, [("me", i)])
                  P.op("dve", lambda e: e.max(out=top8[i][:], in_=me[i][:]), [("me", i)], [("top8", i)])
                  ts("dve", oh[i][:, 0, :], me[i][:], top8[i][:, 0:1], ALU.is_equal, [("me", i), ("top8", i)], [("oh", i)])
                  ts("dve", oh[i][:, 1, :], me[i][:], top8[i][:, 1:2], ALU.is_equal, [("me", i), ("top8", i), ("oh", i)], [("oh", i)])
                  tt("dve", s_[:, 5:6], top8[i][:, 1:2], top8[i][:, 0:1], ALU.subtract, [("top8", i)] + SK, SK)
                  act(s_[:, 5:6], s_[:, 5:6], AF.Exp, SK, SK)
                  ts("dve", s_[:, 5:6], s_[:, 5:6], 1.0, ALU.add, SK, SK)
                  P.op("dve", lambda e: e.reciprocal(out=s_[:, 5:6], in_=s_[:, 5:6]), SK, SK)
                  tt("dve", s_[:, 6:7], s_[:, 5:6], s_[:, 4:5], ALU.mult, SK, SK)
                  tt("dve", s_[:, 7:8], s_[:, 4:5], s_[:, 6:7], ALU.subtract, SK, SK)
                  ts("dve", comb[:, blk, :], oh[i][:, 0, :], s_[:, 6:7], ALU.mult, [("oh", i)] + SK, [("comb", blk)])
                  stt("dve", comb[:, blk, :], oh[i][:, 1, :], s_[:, 7:8], comb[:, blk, :], ALU.mult, ALU.add, [("oh", i), ("comb", blk)] + SK,
                      [("comb", blk)])

              stA1(0)
              for blk in range(16):
                  if blk + 1 < 16:
                      stA1(blk + 1)
                  stA2(blk)
                  stB(blk)
              if debug:
                  P.dma("sp", dbg["comb"], comb[:], reads=[("comb", b) for b in range(16)], writes=["dbg_comb"])
              P.barrier()
              ckpt(6)

          with ExitStack() as s6:
              weg = [sbt(s6, f"weg{i}", [128, 8, 256], BF16) for i in range(2)]
              weu = [sbt(s6, f"weu{i}", [128, 8, 256], BF16) for i in range(2)]
              wed = [sbt(s6, f"wed{i}", [128, 2, 1024], BF16) for i in range(2)]
              stgE = [sbt(s6, f"stgE{i}", [128, 2048]) for i in range(3)]
              sg_ = [sbt(s6, f"sg{i}", [128, 512]) for i in range(2)]
              aT = [sbt(s6, f"aT{i}", [128, 2, 512], BF16) for i in range(2)]
              se = [0]

              def load_e(slot, e_):
                  for j, (src, dst) in enumerate(((w_eg, weg), (w_eu, weu))):
                      i = se[0] % 3; se[0] += 1
                      sv = stgE[i][:].rearrange("p (c n) -> p c n", c=8)
                      P.dma("sp", sv, src[e_].rearrange("(c p) n -> p c n", p=128), writes=[("stgE", i)])
                      cp("pool", dst[slot][:], sv, [("stgE", i)], [("we", slot, j)])
                  i = se[0] % 3; se[0] += 1
                  sv = stgE[i][:].rearrange("p (c n) -> p c n", c=2)
                  P.dma("sp", sv, w_ed[e_].rearrange("(c p) n -> p c n", p=128), writes=[("stgE", i)])
                  cp("pool", wed[slot][:], sv, [("stgE", i)], [("we", slot, 2)])

              load_e(0, 0)
              it = 0; yb = 0
              for e_ in range(NE):
                  slot = e_ % 2
                  if e_ + 1 < NE:
                      load_e(1 - slot, e_ + 1)
                  for G in range(4):
                      ai = it % 2; it += 1
                      for fc in range(2):
                          bg, bu = (0, 1) if fc == 0 else (2, 3)
                          for dc in range(8):
                              mm(ps[bg][:, :], weg[slot][:, dc, fc * 128:(fc + 1) * 128], x1T[:, dc, G * 512:(G + 1) * 512], dc == 0, dc == 7,
                                 [("we", slot, 0)], [PSK[bg]])
                          for dc in range(8):
                              mm(ps[bu][:, :], weu[slot][:, dc, fc * 128:(fc + 1) * 128], x1T[:, dc, G * 512:(G + 1) * 512], dc == 0, dc == 7,
                                 [("we", slot, 1)], [PSK[bu]])
                          act(sg_[fc][:], ps[bg][:, :], AF.Silu, [PSK[bg]], [("sg", fc)])
                          tt("dve", aT[ai][:, fc, :], sg_[fc][:], ps[bu][:, :], ALU.mult, [("sg", fc), PSK[bu]], [("aT", ai, fc)])
                      for b4 in range(4):
                          blk = G * 4 + b4
                          for ch in range(2):
                              bank = 4 + (yb % 4); yb += 1
                              for fc in range(2):
                                  mm(ps[bank][:, :], aT[ai][:, fc, b4 * 128:(b4 + 1) * 128], wed[slot][:, fc, ch * 512:(ch + 1) * 512], fc == 0, fc == 1,
                                     [("aT", ai, 0), ("aT", ai, 1), ("we", slot, 2)], [PSK[bank]])
                              stt("dve", acc[:, blk, ch * 512:(ch + 1) * 512], ps[bank][:, :], comb[:, blk, e_:e_ + 1], acc[:, blk, ch * 512:(ch + 1) * 512],
                                  ALU.mult, ALU.add, [PSK[bank], ("accw", blk, ch)], [("accw", blk, ch)])
              P.barrier()
              ckpt(7)

          with ExitStack() as s7:
              lng2 = sbt(s7, "lng2", [128, 1024]); lnb2 = sbt(s7, "lnb2", [128, 1024])
              ot = [sbt(s7, f"ot{i}", [128, 1024]) for i in range(2)]
              st2 = [sbt(s7, f"st2{i}", [128, 2, 6]) for i in range(2)]
              mv2 = [sbt(s7, f"mv2{i}", [128, 2]) for i in range(2)]
              rs2 = [sbt(s7, f"rs2{i}", [128, 1]) for i in range(2)]
              P.dma("sp", lng2[:], ln_d[2].partition_broadcast(128), writes=["lng2"])
              P.dma("sp", lnb2[:], ln_d[3].partition_broadcast(128), writes=["lnb2"])
              outk = []
              for blk in range(16):
                  i = blk % 2
                  for ch in range(2):
                      P.op("dve", lambda e: e.bn_stats(out=st2[i][:, ch, :], in_=acc[:, blk, ch * 512:(ch + 1) * 512]), [], [("st2", i, ch)])
                  P.op("dve", lambda e: e.bn_aggr(out=mv2[i][:], in_=st2[i][:].rearrange("p a b -> p (a b)")), [("st2", i, 0), ("st2", i, 1)], [("mv2", i)])
                  act(rs2[i][:], mv2[i][:, 1:2], AF.Ln, [("mv2", i)], [("rs2", i)], bias=LN_EPS)
                  act(rs2[i][:], rs2[i][:], AF.Exp, [("rs2", i)], [("rs2", i)], scale=-0.5)
                  ts("dve", ot[i][:], acc[:, blk, :], mv2[i][:, 0:1], ALU.subtract, [("mv2", i), ("rs2", i)], [("ot", i)], s2=rs2[i][:, 0:1], op1=ALU.mult)
                  tt("pool", ot[i][:], ot[i][:], lng2[:], ALU.mult, [("ot", i), "lng2"], [("ot", i)])
                  tt("pool", ot[i][:], ot[i][:], lnb2[:], ALU.add, [("ot", i), "lnb2"], [("ot", i)])
                  P.dma("sp", out_d[blk * 128:(blk + 1) * 128, :], ot[i][:], reads=[("ot", i)], writes=[("out", blk)])
                  outk.append(("out", blk))
              P.finish(outk + [k for k in P.bufs if isinstance(k, str) and k.startswith("dbg")] +
                       [k for k in P.bufs if isinstance(k, tuple) and isinstance(k[0], str) and k[0].startswith("dbg")])
              P.barrier()
    except _Stop:
        pass
    return nc


def _blocks(p):
    own, oth = [], []
    for j in range(8):
        if p == 0:
            own += [4 * j, 4 * j + 3]; oth += [4 * j + 1, 4 * j + 2]
        else:
            own += [4 * j + 1, 4 * j + 2]; oth += [4 * j, 4 * j + 3]
    return own, oth


def _tok(blocks):
    return np.concatenate([np.arange(b * 128, (b + 1) * 128) for b in blocks])


_NC_CACHE = {}


def _prepare(x, mem, w_in, b_forget, b_gates, lambda_q1, lambda_k1, lambda_q2, lambda_k2,
           diff_subln_g, w_mem_kv, w_branch_fox, w_branch_diff, w_branch_mem, w_out,
           ln1_g, ln1_b, w_router_group, b_router_group, w_router_expert, b_router_expert,
           w_expert_gate, w_expert_up, w_expert_down, ln2_g, ln2_b):
    f = lambda a: np.ascontiguousarray(np.asarray(a), dtype=np.float32)
    x = f(x); mem = f(mem)
    shared = {
        "w_in": f(w_in[0]), "w_mkv": f(w_mem_kv[0]),
        "w_bf": f(w_branch_fox[0]), "w_bd": f(w_branch_diff[0]), "w_bm": f(w_branch_mem[0]),
        "w_out": f(w_out[0]),
        "w_r": f(np.concatenate([np.asarray(w_router_group[0]), np.asarray(w_router_expert[0])], axis=1)),
        "w_eg": f(w_expert_gate[0]), "w_eu": f(w_expert_up[0]), "w_ed": f(w_expert_down[0]),
        "nbf": f(-np.asarray(b_forget[0]).reshape(8, 1)),
        "bgt": f(np.asarray(b_gates[0]).reshape(24, 128).T),
        "lam4": f(np.stack([np.asarray(lambda_q1[0]), np.asarray(lambda_k1[0]), np.asarray(lambda_q2[0]), np.asarray(lambda_k2[0])])),
        "subln_g": f(np.asarray(diff_subln_g[0]).reshape(128, 1)),
        "ln1_g": f(ln1_g[0]), "ln1_b": f(ln1_b[0]), "ln2_g": f(ln2_g[0]), "ln2_b": f(ln2_b[0]),
        "b_r": f(np.concatenate([np.asarray(b_router_group[0]), np.asarray(b_router_expert[0])])),
    }
    rst = np.ones((8, S), np.float32); rst[:, ::128] = 0.0
    shared["rst"] = rst
    slopes = [2.0 ** (-8.0 * (h + 1) / 4) for h in range(4)]

    def hi_lo(v):
        hi = np.floor(v / 16.0) * 16.0
        return hi, v - hi

    in_maps = []
    toks = []
    for c in range(NCORES):
        b, p = c // 2, c % 2
        own, oth = _blocks(p)
        to, tr_ = _tok(own), _tok(oth)
        toks.append((b, to))
        m = dict(shared)
        m["xTo"] = f(x[b, to, :].T); m["xTr"] = f(x[b, tr_, :].T); m["xo"] = f(x[b, to, :])
        m["memT"] = f(mem[b].T)
        par = np.zeros((128, 2), np.float32); par[:, 0] = 1.0 - p; par[:, 1] = p
        m["par"] = par
        pos_k = np.concatenate([to, tr_]).astype(np.float64)
        pos_q = to.astype(np.float64)
        dq = np.ones((4, 4, T), np.float32); dk = np.ones((4, 4, S), np.float32)
        for h in range(4):
            hq, lq = hi_lo(pos_q); hk, lk = hi_lo(pos_k)
            dq[h, 0] = -slopes[h] * hq; dq[h, 1] = -slopes[h] * lq
            dk[h, 2] = slopes[h] * hk; dk[h, 3] = slopes[h] * lk
        m["dqaug"] = dq; m["dkaug"] = dk
        in_maps.append(m)
    return in_maps, toks


def kernel(**inputs):
    debug = bool(int(os.environ.get("MK_DEBUG", "0")))
    in_maps, toks = _prepare(**inputs)
    key = debug
    if key not in _NC_CACHE:
        _NC_CACHE[key] = build_program(debug)
    nc = _NC_CACHE[key]
    res = run_bass_kernel_spmd(nc, in_maps, core_ids=list(range(NCORES)))
    out = np.empty((4, S, D), np.float32)
    for c in range(NCORES):
        b, to = toks[c]
        out[b, to, :] = res.results[c]["out"]
    if debug:
        kernel.debug_results = res.results
        kernel.toks = toks
    return out
```

```python
import os
from contextlib import ExitStack
import numpy as np
import concourse.bass as bass
import concourse.mybir as mybir
from concourse.bass_utils import run_bass_kernel_spmd

F32 = mybir.dt.float32
BF16 = mybir.dt.bfloat16
AF = mybir.ActivationFunctionType
ALU = mybir.AluOpType
AX = mybir.AxisListType

NCORES = 8
D = 1024
S = 4096
T = 2048
NE = 32
ALPHA = 2.0 ** 0.25
LN_EPS = 1e-5
OFF = dict(fq=0, fk=512, fv=1024, dq=1536, dk=2048, dv=2560, mq=3072, gl=3584, fl=6656)


class Prog:
    SEM_WRAP = int(os.environ.get("MK_WRAP", "1000"))

    def __init__(self, nc, stack, n_dma_sems=6):
        self.nc = nc
        self.stack = stack
        self.E = {"pe": nc.tensor, "act": nc.scalar, "dve": nc.vector,
                  "pool": nc.gpsimd, "sp": nc.sync}
        self.sems = {}
        self.cur = {}
        self.nsem = 0
        for e in self.E:
            self._new_sem(e)
        self.waited = {e: {} for e in self.E}
        self.bufs = {}
        self.dring = {}
        for q in ("sp", "pool", "act"):
            ring = []
            for i in range(n_dma_sems):
                k = f"d_{q}_{i}"
                h = stack.enter_context(nc.semaphore(k))
                self.sems[k] = h
                ring.append([h, k, 0])
            self.dring[q] = [ring, 0]
        self.ninst = {e: 0 for e in self.E}

    def _new_sem(self, e):
        self.nsem += 1
        k = f"s_{e}_{self.nsem}"
        h = self.stack.enter_context(self.nc.semaphore(k))
        self.cur[e] = [h, k, 0]
        self.sems[k] = h

    def _deps(self, reads, writes):
        deps = {}

        def add(d):
            for k, v in d.items():
                if deps.get(k, 0) < v:
                    deps[k] = v
        for key in reads:
            st = self.bufs.get(key)
            if st:
                add(st[0])
        for key in writes:
            st = self.bufs.get(key)
            if st:
                add(st[0])
                add(st[1])
        return deps

    def _wait(self, eng, deps, skip_self=False):
        e = self.E[eng]
        w = self.waited[eng]
        for k, v in deps.items():
            if skip_self and k.startswith(f"s_{eng}_"):
                continue
            if w.get(k, 0) < v:
                e.wait_ge(self.sems[k], v)
                w[k] = v

    def _mark(self, tok, reads, writes):
        k, v = tok
        for key in reads:
            st = self.bufs.setdefault(key, [{}, {}])
            if st[1].get(k, 0) < v:
                st[1][k] = v
        for key in writes:
            self.bufs[key] = [{k: v}, {}]

    def op(self, eng, fn, reads=(), writes=()):
        deps = self._deps(reads, writes)
        self._wait(eng, deps, skip_self=(eng == "pe"))
        c = self.cur[eng]
        if c[2] >= self.SEM_WRAP:
            self._new_sem(eng)
            c = self.cur[eng]
        ins = fn(self.E[eng])
        ins.then_inc(c[0], 1)
        c[2] += 1
        self.ninst[eng] += 1
        self._mark((c[1], c[2]), reads, writes)

    def dma(self, q, out, in_, reads=(), writes=(), **kw):
        deps = self._deps(reads, writes)
        ring, idx = self.dring[q]
        s = ring[idx % len(ring)]
        self.dring[q][1] = idx + 1
        if s[2] > 0:
            deps[s[1]] = max(deps.get(s[1], 0), s[2])
        self._wait(q, deps)
        ins = self.E[q].dma_start(out=out, in_=in_, **kw)
        s[2] += 16
        ins.then_inc(s[0], 16)
        self._mark((s[1], s[2]), reads, writes)

    def barrier(self):
        toks = {}
        for e in self.E:
            c = self.cur[e]
            if c[2] > 0:
                toks[c[1]] = c[2]
        for q in self.dring:
            for s in self.dring[q][0]:
                if s[2] > 0:
                    toks[s[1]] = s[2]
        for e in self.E:
            self._wait(e, dict(toks), skip_self=True)
        self.bufs = {}

    def finish(self, keys, eng="sp"):
        deps = self._deps(keys, ())
        self._wait(eng, deps)


def attn_steps(G):
    steps = []
    for j in range(2 * G):
        for kb in (2 * j, 2 * j + 1, 16 + 2 * j, 16 + 2 * j + 1):
            steps.append((kb, 0, None))
    b = 4 * G
    steps += [(b, 0, "tri"), (16 + b, 0, "par1"), (b + 1, 128, "tri"), (16 + b + 1, 128, "par0"),
              (b + 2, 256, "tri"), (16 + b + 2, 256, "par1"), (b + 3, 384, "tri"), (16 + b + 3, 384, "par0")]
    return steps


def build_program(debug=False):
    nc = bass.Bass("TRN2", target_bir_lowering=False)

    def din(name, shape, dt=F32):
        return nc.dram_tensor(name, list(shape), dt, kind="ExternalInput").ap()

    xTo_d = din("xTo", [D, T]); xTr_d = din("xTr", [D, T]); xo_d = din("xo", [T, D])
    memT_d = din("memT", [D, 256])
    w_in = din("w_in", [D, 6664]); w_mkv = din("w_mkv", [D, 1024])
    w_br_d = [din("w_bf", [512, D]), din("w_bd", [512, D]), din("w_bm", [512, D])]
    w_out_d = din("w_out", [D, D]); w_r_d = din("w_r", [D, 36])
    w_eg = din("w_eg", [NE, D, 256]); w_eu = din("w_eu", [NE, D, 256]); w_ed = din("w_ed", [NE, 256, D])
    nbf_d = din("nbf", [8, 1]); bgt_d = din("bgt", [128, 24]); lam_d = din("lam4", [4, 64])
    sg_d = din("subln_g", [128, 1])
    ln_d = [din("ln1_g", [D]), din("ln1_b", [D]), din("ln2_g", [D]), din("ln2_b", [D])]
    b_r_d = din("b_r", [36])
    par_d = din("par", [128, 2]); dqa_d = din("dqaug", [4, 4, T]); dka_d = din("dkaug", [4, 4, S])
    rst_d = din("rst", [8, S])
    out_d = nc.dram_tensor("out", [T, D], F32, kind="ExternalOutput").ap()
    scr_k = nc.dram_tensor("scr_k", [8, 3, S], BF16, kind="Internal").ap()
    scr_q = nc.dram_tensor("scr_q", [8, 3, T], BF16, kind="Internal").ap()
    dbg = {}
    if debug:
        dbg["yT"] = nc.dram_tensor("dbg_yT", [128, 12, T], BF16, kind="ExternalOutput").ap()
        dbg["x1"] = nc.dram_tensor("dbg_x1", [T, D], F32, kind="ExternalOutput").ap()
        dbg["comb"] = nc.dram_tensor("dbg_comb", [128, 16, NE], F32, kind="ExternalOutput").ap()

    STOP = int(os.environ.get("MK_STOP", "99"))

    class _Stop(Exception):
        pass

    def ckpt(n):
        if STOP == n:
            P.barrier()
            raise _Stop()

    try:
      with ExitStack() as top:
          P = Prog(nc, top)

          def sbt(st, name, shape, dt=F32):
              return st.enter_context(nc.sbuf_tensor("sb_" + name, list(shape), dt))

          def mm(out, lhsT, rhs, start, stop, reads, writes):
              P.op("pe", lambda e: e.matmul(out, lhsT=lhsT, rhs=rhs, start=start, stop=stop), reads, writes)

          def tr(out, in_, reads, writes):
              P.op("pe", lambda e: e.transpose(out=out, in_=in_, identity=ident[:]), list(reads) + ["ident"], writes)

          def act(out, in_, func, reads, writes, bias=None, scale=None, accum_out=None):
              kw = {}
              if bias is not None:
                  kw["bias"] = bias
              if scale is not None:
                  kw["scale"] = scale
              if accum_out is not None:
                  kw["accum_out"] = accum_out
              P.op("act", lambda e: e.activation(out=out, in_=in_, func=func, **kw), reads, writes)

          def cp(eng, out, in_, reads, writes):
              if eng == "act":
                  P.op("act", lambda e: e.activation(out=out, in_=in_, func=AF.Copy), reads, writes)
              else:
                  P.op(eng, lambda e: e.tensor_copy(out=out, in_=in_), reads, writes)

          def tt(eng, out, in0, in1, op, reads, writes):
              P.op(eng, lambda e: e.tensor_tensor(out=out, in0=in0, in1=in1, op=op), reads, writes)

          def ts(eng, out, in0, s1, op0, reads, writes, s2=None, op1=None):
              if op1 is None:
                  P.op(eng, lambda e: e.tensor_scalar(out=out, in0=in0, scalar1=s1, scalar2=None, op0=op0), reads, writes)
              else:
                  P.op(eng, lambda e: e.tensor_scalar(out=out, in0=in0, scalar1=s1, scalar2=s2, op0=op0, op1=op1), reads, writes)

          def stt(eng, out, in0, scalar, in1, op0, op1, reads, writes):
              P.op(eng, lambda e: e.scalar_tensor_tensor(out=out, in0=in0, scalar=scalar, in1=in1, op0=op0, op1=op1), reads, writes)

          def memset(eng, ap, val, writes):
              P.op(eng, lambda e: e.memset(ap, val), (), writes)

          ps = [top.enter_context(nc.psum_tensor(f"ps{i}", [128, 512], F32)) for i in range(8)]
          PSK = [("ps", i) for i in range(8)]
          bufA = sbt(top, "bufA", [128, 8, T], BF16)
          bufB = sbt(top, "bufB", [128, 8, T], BF16)
          bufC = sbt(top, "bufC", [128, 16384], F32)
          bufCb = bufC[:].bitcast(BF16)
          yT = bufCb[:, 0:24576].rearrange("p (c t) -> p c t", c=12)
          KT = bufCb[:, 24576:32768].rearrange("p (h t) -> p h t", h=2)
          acc = bufC[:].rearrange("p (b d) -> p b d", b=16)
          xTo = bufA; xTr = bufB; x1T = bufA; hT = bufB
          ident = sbt(top, "ident", [128, 128]); onesb = sbt(top, "onesb", [128, 128], BF16)
          onesf = sbt(top, "onesf", [128, 64]); par = sbt(top, "par_sb", [128, 2])
          comb = sbt(top, "comb", [128, 16, NE])
          nlam = sbt(top, "nlam", [128, 1]); gs = sbt(top, "gs", [128, 1])

          memset("pool", ident[:], 1.0, ["ident"])
          P.op("pool", lambda e: e.affine_select(out=ident[:], in_=ident[:], pattern=[[-1, 128]], compare_op=ALU.is_equal,
                                                 fill=0.0, base=0, channel_multiplier=1), ["ident"], ["ident"])
          memset("pool", onesb[:], 1.0, ["onesb"])
          identb = sbt(top, "identb", [128, 128], BF16); Ttri = sbt(top, "Ttri", [128, 128], BF16)
          Zp = [sbt(top, f"Zp{i}", [128, 128], BF16) for i in range(2)]
          cp("pool", identb[:], ident[:], ["ident"], ["identb"])
          memset("pool", Ttri[:], 0.0, ["Ttri"])
          P.op("pool", lambda e: e.affine_select(out=Ttri[:], in_=Ttri[:], pattern=[[1, 128]], compare_op=ALU.is_ge,
                                                 fill=-30000.0, base=0, channel_multiplier=-1), ["Ttri"], ["Ttri"])
          memset("pool", onesf[:], 1.0, ["onesf"])
          P.dma("sp", par[:], par_d, writes=["par"])
          for i_ in range(2):
              ts("dve", Zp[i_][:], onesb[:], par[:, i_:i_ + 1], ALU.mult, ["onesb", "par"], [("Zp", i_)], s2=-1.0, op1=ALU.add)
              ts("dve", Zp[i_][:], Zp[i_][:], 30000.0, ALU.mult, [("Zp", i_)], [("Zp", i_)])

          for dc in range(8):
              P.dma("pool", xTo[:, dc, :], xTo_d[dc * 128:(dc + 1) * 128, :], writes=[("xTo", dc)])
          for dc in range(8):
              P.dma("pool", xTr[:, dc, :], xTr_d[dc * 128:(dc + 1) * 128, :], writes=[("xTr", dc)])
          XTO = [("xTo", dc) for dc in range(8)]
          XTR = [("xTr", dc) for dc in range(8)]
          ckpt(0)

          with ExitStack() as s1:
              pk = sbt(s1, "pk", [8, 3, S], BF16); pq = sbt(s1, "pq", [8, 3, T], BF16)
              wflf = sbt(s1, "wflf", [128, 8, 8]); wfl = sbt(s1, "wfl", [128, 8, 8], BF16)
              nbf = sbt(s1, "nbf_sb", [8, 1]); tot = sbt(s1, "tot", [8, 32]); base = sbt(s1, "base", [8, 32])
              sm = sbt(s1, "sm", [8, 4, 8])
              lamt = sbt(s1, "lamt", [128, 4, 64]); lame = sbt(s1, "lame", [128, 2]); lamp = sbt(s1, "lamp", [128, 2, 64])
              sgt = sbt(s1, "sgt", [128, 1])
              spv = bufC[0:8, 0:4096]; wv_ = bufC[0:8, 4096:8192]; rstv = bufC[0:8, 8192:12288]
              P.dma("sp", wflf[:], w_in[:, OFF["fl"]:OFF["fl"] + 8].rearrange("(c p) n -> p c n", p=128), writes=["wflf"])
              P.dma("sp", nbf[:], nbf_d, writes=["nbf"])
              P.dma("sp", rstv, rst_d, writes=["rst"])
              cp("dve", wfl[:], wflf[:], ["wflf"], ["wfl"])
              for i in range(4):
                  P.dma("sp", lamt[:, i, :], lam_d[i].partition_broadcast(128), writes=[("lamt", i)])
              P.dma("sp", sgt[:], sg_d, writes=["sgt"])
              tt("dve", lamp[:, 0, :], lamt[:, 0, :], lamt[:, 1, :], ALU.mult, [("lamt", 0), ("lamt", 1)], ["lamp0"])
              tt("dve", lamp[:, 1, :], lamt[:, 2, :], lamt[:, 3, :], ALU.mult, [("lamt", 2), ("lamt", 3)], ["lamp1"])
              P.op("dve", lambda e: e.reduce_sum(out=lame[:, 0:1], in_=lamp[:, 0, :], axis=AX.X), ["lamp0"], ["lame0"])
              P.op("dve", lambda e: e.reduce_sum(out=lame[:, 1:2], in_=lamp[:, 1, :], axis=AX.X), ["lamp1"], ["lame1"])
              act(lame[:], lame[:], AF.Exp, ["lame0", "lame1"], ["lame"])
              tt("dve", nlam[:], lame[:, 1:2], lame[:, 0:1], ALU.subtract, ["lame"], ["nlam"])
              ts("dve", nlam[:], nlam[:], -0.2, ALU.add, ["nlam"], ["nlam"])
              ts("dve", gs[:], sgt[:], 0.8, ALU.mult, ["sgt"], ["gs"])

              for g in range(8):
                  src, keys = (xTo, XTO) if g < 4 else (xTr, XTR)
                  bank = g % 2
                  for dc in range(8):
                      mm(ps[bank][0:8, :], wfl[:, dc, :], src[:, dc, (g % 4) * 512:(g % 4 + 1) * 512], dc == 0, dc == 7,
                         ["wfl", keys[dc]], [PSK[bank]])
                  sl = spv[:, g * 512:(g + 1) * 512]
                  act(sl, ps[bank][0:8, :], AF.Exp, [PSK[bank], "nbf"], [("sp", g)], bias=nbf[:], scale=-1.0)
                  act(sl, sl, AF.Ln, [("sp", g)], [("sp", g)], bias=1.0)
              SPK = [("sp", g) for g in range(8)]
              P.op("dve", lambda e: e.tensor_tensor_scan(out=wv_, data0=rstv, data1=spv, initial=0.0, op0=ALU.mult, op1=ALU.add),
                   SPK + ["rst"], ["w"])
              cp("dve", tot[:], wv_.rearrange("p (b i) -> p b i", i=128)[:, :, 127], ["w"], ["tot"])
              tv = tot[:].rearrange("p (s j t) -> p s j t", s=2, t=2)
              bv = base[:].rearrange("p (s j t) -> p s j t", s=2, t=2)
              tA, tB, tC, tD = tv[:, 0, :, 0], tv[:, 0, :, 1], tv[:, 1, :, 0], tv[:, 1, :, 1]
              Tj, AC, incl, Base = sm[:, 0, :], sm[:, 1, :], sm[:, 2, :], sm[:, 3, :]
              tt("dve", Tj, tA, tB, ALU.add, ["tot"], ["Tj"])
              tt("dve", AC, tC, tD, ALU.add, ["tot"], ["AC"])
              tt("dve", Tj, Tj, AC, ALU.add, ["Tj", "AC"], ["Tj"])
              P.op("dve", lambda e: e.tensor_tensor_scan(out=incl, data0=onesf[0:8, 0:8], data1=Tj, initial=0.0, op0=ALU.mult, op1=ALU.add),
                   ["Tj", "onesf"], ["incl"])
              tt("dve", Base, incl, Tj, ALU.subtract, ["incl", "Tj"], ["Base"])
              p0, p1 = par[0:8, 0:1], par[0:8, 1:2]
              stt("dve", bv[:, 0, :, 0], tC, p1, Base, ALU.mult, ALU.add, ["tot", "Base", "par"], ["bA"])
              stt("dve", bv[:, 1, :, 0], tA, p0, Base, ALU.mult, ALU.add, ["tot", "Base", "par"], ["bC"])
              tt("dve", AC, tA, tC, ALU.add, ["tot", "AC"], ["AC"])
              tt("dve", AC, AC, Base, ALU.add, ["AC", "Base"], ["AC"])
              stt("dve", bv[:, 0, :, 1], tD, p0, AC, ALU.mult, ALU.add, ["tot", "AC", "par"], ["bB"])
              stt("dve", bv[:, 1, :, 1], tB, p1, AC, ALU.mult, ALU.add, ["tot", "AC", "par"], ["bD"])
              for blk in range(32):
                  sl = wv_[:, blk * 128:(blk + 1) * 128]
                  ts("dve", sl, sl, base[:, blk:blk + 1], ALU.add, ["w", "bA", "bB", "bC", "bD"], ["w"])
              cp("dve", pk[:, 0, :], wv_, ["w"], ["pk0"])
              tt("dve", spv, wv_, pk[:, 0, :], ALU.subtract, ["w", "pk0"] + SPK, ["r"])
              cp("dve", pk[:, 1, :], spv, ["r"], ["pk1"])
              tt("dve", spv, spv, pk[:, 1, :], ALU.subtract, ["r", "pk1"], ["r"])
              cp("dve", pk[:, 2, :], spv, ["r"], ["pk2"])
              ts("dve", pq[:], pk[:, :, 0:T], -1.0, ALU.mult, ["pk0", "pk1", "pk2"], ["pq"])
              P.dma("sp", scr_k, pk[:], reads=["pk0", "pk1", "pk2"], writes=["scr_k"])
              P.dma("sp", scr_q, pq[:], reads=["pq"], writes=["scr_q"])
              P.barrier()
              ckpt(1)
          P.bufs["scr_k"] = [{}, {}]

          with ExitStack() as s2:
              QT = sbt(s2, "QT", [128, 2, T], BF16)
              Vb = sbt(s2, "Vb", [128, 32, 132], BF16)
              PT = [sbt(s2, f"PT{i}", [128, 512], BF16) for i in range(4)]
              wsl = [[sbt(s2, f"wsl{i}{j}", [128, 8, 128], BF16) for j in range(3)] for i in range(2)]
              stg = [sbt(s2, f"stg{i}", [128, 8, 128]) for i in range(3)]
              denrow = [sbt(s2, f"denrow{i}", [128, 512]) for i in range(2)]
              rec = [sbt(s2, f"rec{i}", [128, 512]) for i in range(2)]
              t1 = sbt(s2, "t1", [128, 512]); t2 = sbt(s2, "t2", [128, 512]); sqb = sbt(s2, "sqb", [128, 512], BF16)
              stgc = [0]

              def load_w(dst, dkey, col0):
                  i = stgc[0] % 3
                  stgc[0] += 1
                  P.dma("sp", stg[i][:], w_in[:, col0:col0 + 128].rearrange("(c p) n -> p c n", p=128), writes=[("stg", i)])
                  cp("pool", dst[:], stg[i][:], [("stg", i)], [dkey])

              def proj_KQ(slot, bank0, qscale_eng="act"):
                  wk, wq = wsl[slot][0], wsl[slot][1]
                  bi = 0
                  for g in range(8):
                      src, keys = (xTo, XTO) if g < 4 else (xTr, XTR)
                      bank = bank0 + (bi % 2); bi += 1
                      for dc in range(8):
                          mm(ps[bank][:, :], wk[:, dc, :], src[:, dc, (g % 4) * 512:(g % 4 + 1) * 512], dc == 0, dc == 7,
                             [("wsl", slot, 0), keys[dc]], [PSK[bank]])
                      cp("dve", KT[0:64, 0, g * 512:(g + 1) * 512], ps[bank][0:64, :], [PSK[bank]], [("KT", 0, g)])
                      cp("act", KT[0:64, 1, g * 512:(g + 1) * 512], ps[bank][64:128, :], [PSK[bank]], [("KT", 1, g)])
                  for g in range(4):
                      bank = bank0 + (bi % 2); bi += 1
                      for dc in range(8):
                          mm(ps[bank][:, :], wq[:, dc, :], xTo[:, dc, g * 512:(g + 1) * 512], dc == 0, dc == 7,
                             [("wsl", slot, 1), XTO[dc]], [PSK[bank]])
                      ts("dve", QT[0:64, 0, g * 512:(g + 1) * 512], ps[bank][0:64, :], 0.125, ALU.mult, [PSK[bank]], [("QT", 0, g)])
                      act(QT[0:64, 1, g * 512:(g + 1) * 512], ps[bank][64:128, :], AF.Copy, [PSK[bank]], [("QT", 1, g)], scale=0.125)

              def proj_V(slot, bank0, fox):
                  wvv = wsl[slot][2]
                  for b4 in range(8):
                      bank = bank0 + (b4 % 2)
                      for bb in range(4):
                          blk = b4 * 4 + bb
                          src, keys = (xTo, XTO) if blk < 16 else (xTr, XTR)
                          for dc in range(8):
                              mm(ps[bank][:, bb * 128:(bb + 1) * 128], src[:, dc, (blk % 16) * 128:(blk % 16 + 1) * 128], wvv[:, dc, :],
                                 dc == 0, dc == 7, [("wsl", slot, 2), keys[dc]], [PSK[bank]])
                      pv = ps[bank][:, :].rearrange("p (b c) -> p b c", b=4)
                      if fox:
                          cp("dve", Vb[:, b4 * 4:(b4 + 1) * 4, 0:64], pv[:, :, 0:64], [PSK[bank]], [("V", 0, b4)])
                          cp("dve", Vb[:, b4 * 4:(b4 + 1) * 4, 66:130], pv[:, :, 64:128], [PSK[bank]], [("V", 1, b4)])
                      else:
                          eng = "dve" if b4 % 2 == 0 else "act"
                          cp(eng, Vb[:, b4 * 4:(b4 + 1) * 4, 0:128], pv, [PSK[bank]], [("V", 0, b4), ("V", 1, b4)])

              def maskmm(bank, kind):
                  M, mk = {"tri": (Ttri, "Ttri"), "par0": (Zp[0], ("Zp", 0)), "par1": (Zp[1], ("Zp", 1))}[kind]
                  mm(ps[bank][:, 0:128], identb[:, :], M[:, :], False, True, ["identb", mk], [PSK[bank]])

              KTk = lambda hh, kb: [("KT", hh, kb // 4), ("KTa", hh)]
              QTk = lambda hh, G: [("QT", hh, G), ("QTa", hh)]

              memset("dve", KT[64:70, :, :], 1.0, [("KTa", 0), ("KTa", 1)])
              memset("dve", QT[64:70, :, :], 1.0, [("QTa", 0), ("QTa", 1)])
              memset("pool", Vb[:, :, 64:65], 1.0, [("Vone", 0)])
              memset("pool", Vb[:, :, 130:131], 1.0, [("Vone", 1)])
              for j, nm in enumerate(("fk", "fq", "fv")):
                  load_w(wsl[0][j], ("wsl", 0, j), OFF[nm])
              sctr = [0]; pctr = [0]; actr = [0]; pend = []
              for hp in range(4):
                  slot = hp % 2
                  if hp + 1 < 4:
                      for j, nm in enumerate(("fk", "fq", "fv")):
                          load_w(wsl[1 - slot][j], ("wsl", 1 - slot, j), OFF[nm] + (hp + 1) * 128)
                  ckpt(20)
                  proj_KQ(slot, 6)
                  ckpt(21)
                  proj_V(slot, 6, True)
                  ckpt(22)
                  for hh in range(2):
                      h = 2 * hp + hh
                      P.dma("sp", KT[67:70, hh, :], scr_k[h], reads=["scr_k"], writes=[("KTa", hh)])
                      P.dma("sp", QT[64:67, hh, :], scr_q[h], reads=["scr_q"], writes=[("QTa", hh)])
                  ckpt(23)
                  for hh in range(2):
                      for G in range(4):
                          if G == 1:
                              ckpt(25)
                          steps = attn_steps(G)
                          n = len(steps)
                          ab = 3 + (actr[0] % 2); actr[0] += 1
                          L = 3
                          slots = {}
                          for i in range(n + L):
                              if i < n:
                                  kb, c0, spc = steps[i]
                                  w = 512 - c0
                                  sb_ = (0, 1, 2, 7)[sctr[0] % 4]; sctr[0] += 1
                                  pb = pctr[0] % 4; pctr[0] += 1
                                  slots[i] = (sb_, pb)
                                  mm(ps[sb_][:, 0:w], KT[0:70, hh, kb * 128:(kb + 1) * 128], QT[0:70, hh, G * 512 + c0:(G + 1) * 512],
                                     True, spc is None, KTk(hh, kb) + QTk(hh, G), [PSK[sb_]])
                                  if spc:
                                      maskmm(sb_, spc)
                                  act(PT[pb][:, 0:w], ps[sb_][:, 0:w], AF.Exp, [PSK[sb_]], [("PT", pb)])
                              if i == min(4, n + L - 1) and pend:
                                  pend.pop(0)()
                              if i - L >= 0:
                                  ii = i - L
                                  kb, c0, spc = steps[ii]
                                  w = 512 - c0
                                  _, pb = slots[ii]
                                  mm(ps[ab][0:65, c0:512], Vb[:, kb, hh * 66:hh * 66 + 65], PT[pb][:, 0:w], ii == 0, ii == n - 1,
                                     [("V", hh, kb // 4), ("Vone", hh), ("PT", pb)], [PSK[ab]])
                          ckpt(24)
                          d = ab - 3
                          cp("dve", denrow[d][64:65, :], ps[ab][64:65, :], [PSK[ab]], [("denrow", d)])

                          def fin(d=d, ab=ab, hh=hh, hp=hp, G=G):
                              mm(ps[5][0:64, :], onesf[64:65, 0:64], denrow[d][64:65, :], True, True, [("denrow", d), "onesf"], [PSK[5]])
                              P.op("dve", lambda e: e.reciprocal(out=rec[d][0:64, :], in_=ps[5][0:64, :]), [PSK[5]], [("rec", d)])
                              tt("dve", yT[hh * 64:(hh + 1) * 64, hp, G * 512:(G + 1) * 512], ps[ab][0:64, :], rec[d][0:64, :], ALU.mult,
                                 [PSK[ab], ("rec", d)], [("yT", hp, G, hh)])
                          pend.append(fin)

              while pend:
                  pend.pop(0)()
              ckpt(2)
              for j, nm in enumerate(("dk", "dq", "dv")):
                  load_w(wsl[0][j], ("wsl", 0, j), OFF[nm])
              for h in range(4):
                  slot = h % 2
                  if h + 1 < 4:
                      for j, nm in enumerate(("dk", "dq", "dv")):
                          load_w(wsl[1 - slot][j], ("wsl", 1 - slot, j), OFF[nm] + (h + 1) * 128)
                  proj_KQ(slot, 0)
                  proj_V(slot, 2, False)
                  for hh in range(2):
                      for half in range(2):
                          P.dma("pool", KT[64:68, hh, half * 2048:(half + 1) * 2048], dka_d[h, :, half * 2048:(half + 1) * 2048],
                                writes=[("KTa", hh)] if half == 1 else [("KTa", hh)])
                      P.dma("pool", QT[64:68, hh, :], dqa_d[h], writes=[("QTa", hh)])
                  for G in range(4):
                      steps = attn_steps(G)
                      n = len(steps)
                      L = 1
                      slots = {}
                      for i in range(n + L):
                          if i < n:
                              kb, c0, spc = steps[i]
                              w = 512 - c0
                              sl_ = []
                              for hh in range(2):
                                  sb_ = sctr[0] % 4; sctr[0] += 1
                                  pb = pctr[0] % 4; pctr[0] += 1
                                  sl_.append((sb_, pb))
                                  mm(ps[sb_][:, 0:w], KT[0:68, hh, kb * 128:(kb + 1) * 128], QT[0:68, hh, G * 512 + c0:(G + 1) * 512],
                                     True, spc is None, KTk(hh, kb) + QTk(hh, G), [PSK[sb_]])
                                  if spc:
                                      maskmm(sb_, spc)
                                  act(PT[pb][:, 0:w], ps[sb_][:, 0:w], AF.Exp, [PSK[sb_]], [("PT", pb)])
                              slots[i] = sl_
                          if i - L >= 0:
                              ii = i - L
                              kb, c0, spc = steps[ii]
                              w = 512 - c0
                              for hh in range(2):
                                  _, pb = slots[ii][hh]
                                  mm(ps[4 + 2 * hh][:, c0:512], Vb[:, kb, 0:128], PT[pb][:, 0:w], ii == 0, ii == n - 1,
                                     [("V", 0, kb // 4), ("V", 1, kb // 4), ("PT", pb)], [PSK[4 + 2 * hh]])
                                  mm(ps[5 + 2 * hh][:, c0:512], onesb[:, :], PT[pb][:, 0:w], ii == 0, ii == n - 1,
                                     ["onesb", ("PT", pb)], [PSK[5 + 2 * hh]])
                      P.op("dve", lambda e: e.reciprocal(out=t1[:], in_=ps[5][:, :]), [PSK[5]], ["t1"])
                      tt("dve", t1[:], ps[4][:, :], t1[:], ALU.mult, [PSK[4], "t1"], ["t1"])
                      P.op("dve", lambda e: e.reciprocal(out=t2[:], in_=ps[7][:, :]), [PSK[7]], ["t2"])
                      tt("dve", t2[:], ps[6][:, :], t2[:], ALU.mult, [PSK[6], "t2"], ["t2"])
                      stt("dve", t1[:], t2[:], nlam[:], t1[:], ALU.mult, ALU.add, ["t1", "t2", "nlam"], ["t1"])
                      tt("dve", sqb[:], t1[:], t1[:], ALU.mult, ["t1"], ["sqb"])
                      mb = sctr[0] % 4; sctr[0] += 1
                      mm(ps[mb][:, :], onesb[:, :], sqb[:], True, True, ["onesb", "sqb"], [PSK[mb]])
                      act(t2[:], ps[mb][:, :], AF.Ln, [PSK[mb]], ["t2"], bias=LN_EPS, scale=1.0 / 128.0)
                      act(t2[:], t2[:], AF.Exp, ["t2"], ["t2"], scale=-0.5)
                      tt("dve", t1[:], t1[:], t2[:], ALU.mult, ["t1", "t2"], ["t1"])
                      ts("dve", yT[:, 4 + h, G * 512:(G + 1) * 512], t1[:], gs[:], ALU.mult, ["t1", "gs"], [("yT", 4 + h, G)])
              P.barrier()
              ckpt(3)

          with ExitStack() as s3:
              memTs = sbt(s3, "memTs", [128, 8, 256], BF16)
              wm = [sbt(s3, f"wm{i}", [128, 8, 512], BF16) for i in range(3)]
              stg8 = [sbt(s3, f"stg8_{i}", [128, 8, 256]) for i in range(2)]
              mkT = sbt(s3, "mkT", [128, 4, 256], BF16); mv = sbt(s3, "mv", [128, 2, 512], BF16)
              mqs = [sbt(s3, f"mqs{i}", [128, 512], BF16) for i in range(2)]
              PTm = [sbt(s3, f"PTm{i}", [128, 512], BF16) for i in range(4)]
              recm = sbt(s3, "recm", [128, 512])
              for dc in range(8):
                  P.dma("pool", memTs[:, dc, :], memT_d[dc * 128:(dc + 1) * 128, :], writes=[("memT", dc)])
              MK = [("memT", dc) for dc in range(8)]
              k8 = 0
              for i, (srcw, c0) in enumerate(((w_mkv, 0), (w_mkv, 512), (w_in, OFF["mq"]))):
                  for hf in range(2):
                      si = k8 % 2; k8 += 1
                      P.dma("sp", stg8[si][:], srcw[:, c0 + hf * 256:c0 + (hf + 1) * 256].rearrange("(c p) n -> p c n", p=128),
                            writes=[("stg8", si)])
                      cp("pool", wm[i][:, :, hf * 256:(hf + 1) * 256], stg8[si][:], [("stg8", si)], [("wm", i, hf)])
              WM = lambda i: [("wm", i, 0), ("wm", i, 1)]
              for h in range(4):
                  for dc in range(8):
                      mm(ps[0][:, 0:256], wm[0][:, dc, h * 128:(h + 1) * 128], memTs[:, dc, :], dc == 0, dc == 7, WM(0) + [MK[dc]], [PSK[0]])
                  cp("dve", mkT[:, h, :], ps[0][:, 0:256], [PSK[0]], [("mkT", h)])
              for mb in range(2):
                  for dc in range(8):
                      mm(ps[1][:, :], memTs[:, dc, mb * 128:(mb + 1) * 128], wm[1][:, dc, :], dc == 0, dc == 7, WM(1) + [MK[dc]], [PSK[1]])
                  cp("dve", mv[:, mb, :], ps[1][:, :], [PSK[1]], [("mv", mb)])
              def stM1(n_):
                  h, G, qb = n_ // 4, n_ % 4, n_ % 2
                  bq = 0 + qb
                  for dc in range(8):
                      mm(ps[bq][:, :], wm[2][:, dc, h * 128:(h + 1) * 128], xTo[:, dc, G * 512:(G + 1) * 512], dc == 0, dc == 7,
                         WM(2) + [XTO[dc]], [PSK[bq]])
                  act(mqs[qb][:], ps[bq][:, :], AF.Copy, [PSK[bq]], [("mqs", qb)], scale=128.0 ** -0.5)

              def stM2(n_):
                  h, G, qb = n_ // 4, n_ % 4, n_ % 2
                  bn_, bd_ = 4 + 2 * qb, 5 + 2 * qb
                  for mb in range(2):
                      sbk = 2 + mb
                      pb = (2 * qb + mb)
                      mm(ps[sbk][:, :], mkT[:, h, mb * 128:(mb + 1) * 128], mqs[qb][:], True, True, [("mkT", h), ("mqs", qb)], [PSK[sbk]])
                      act(PTm[pb][:], ps[sbk][:, :], AF.Exp, [PSK[sbk]], [("PTm", pb)])
                  for mb in range(2):
                      pb = (2 * qb + mb)
                      mm(ps[bn_][:, :], mv[:, mb, h * 128:(h + 1) * 128], PTm[pb][:], mb == 0, mb == 1, [("mv", mb), ("PTm", pb)], [PSK[bn_]])
                      mm(ps[bd_][:, :], onesb[:, :], PTm[pb][:], mb == 0, mb == 1, ["onesb", ("PTm", pb)], [PSK[bd_]])
                  P.op("dve", lambda e: e.reciprocal(out=recm[:], in_=ps[bd_][:, :]), [PSK[bd_]], ["recm"])
                  tt("dve", yT[:, 8 + h, G * 512:(G + 1) * 512], ps[bn_][:, :], recm[:], ALU.mult, [PSK[bn_], "recm"], [("yT", 8 + h, G)])

              stM1(0)
              for n_ in range(16):
                  if n_ + 1 < 16:
                      stM1(n_ + 1)
                  stM2(n_)
              if debug:
                  P.dma("sp", dbg["yT"], yT, reads=[k for k in P.bufs if isinstance(k, tuple) and k[0] == "yT"], writes=["dbg_yT"])
              P.barrier()
              ckpt(4)

          with ExitStack() as s4:
              wgl = [[sbt(s4, f"wgl{i}{b}", [128, 8, 128], BF16) for b in range(3)] for i in range(2)]
              wbr = [[sbt(s4, f"wbr{i}{b}", [128, 4, 128], BF16) for b in range(3)] for i in range(2)]
              stgA = [sbt(s4, f"stgA{i}", [128, 8, 128]) for i in range(3)]
              bgt = sbt(s4, "bgt", [128, 24])
              gsb = [sbt(s4, f"gsb{i}", [128, 512]) for i in range(2)]
              hac = [sbt(s4, f"hac{i}", [128, 512]) for i in range(2)]
              P.dma("sp", bgt[:], bgt_d, writes=["bgt"])
              sa = [0]

              def load_fc(slot, fc):
                  for br in range(3):
                      i = sa[0] % 3; sa[0] += 1
                      c0 = OFF["gl"] + br * 1024 + fc * 128
                      P.dma("sp", stgA[i][:], w_in[:, c0:c0 + 128].rearrange("(c p) n -> p c n", p=128), writes=[("stgA", i)])
                      cp("pool", wgl[slot][br][:], stgA[i][:], [("stgA", i)], [("wgl", slot, br)])
                      i = sa[0] % 3; sa[0] += 1
                      P.dma("sp", stgA[i][:, 0:4, :], w_br_d[br][:, fc * 128:(fc + 1) * 128].rearrange("(c p) n -> p c n", p=128),
                            writes=[("stgA", i)])
                      cp("pool", wbr[slot][br][:], stgA[i][:, 0:4, :], [("stgA", i)], [("wbr", slot, br)])

              load_fc(0, 0)
              it = 0
              for fc in range(8):
                  slot = fc % 2
                  if fc + 1 < 8:
                      load_fc(1 - slot, fc + 1)
                  for G in range(4):
                      hb = it % 2; it += 1
                      for br in range(3):
                          zb = (br % 2); gb = 2 + (br % 2)
                          if br == 2:
                              zb, gb = 4, 5
                          for kc in range(4):
                              mm(ps[zb][:, :], wbr[slot][br][:, kc, :], yT[:, br * 4 + kc, G * 512:(G + 1) * 512], kc == 0, kc == 3,
                                 [("wbr", slot, br)], [PSK[zb]])
                          for dc in range(8):
                              mm(ps[gb][:, :], wgl[slot][br][:, dc, :], xTo[:, dc, G * 512:(G + 1) * 512], dc == 0, dc == 7,
                                 [("wgl", slot, br)], [PSK[gb]])
                          gt = gsb[br % 2]
                          act(gt[:], ps[gb][:, :], AF.Sigmoid, [PSK[gb], "bgt"], [("gsb", br % 2)], bias=bgt[:, br * 8 + fc:br * 8 + fc + 1])
                          if br == 0:
                              tt("dve", hac[hb][:], gt[:], ps[zb][:, :], ALU.mult, [("gsb", 0), PSK[zb]], [("hac", hb)])
                          else:
                              tt("dve", gt[:], gt[:], ps[zb][:, :], ALU.mult, [("gsb", br % 2), PSK[zb]], [("gsb", br % 2)])
                              if br == 1:
                                  tt("dve", hac[hb][:], hac[hb][:], gt[:], ALU.add, [("hac", hb), ("gsb", 1)], [("hac", hb)])
                              else:
                                  tt("dve", hT[:, fc, G * 512:(G + 1) * 512], hac[hb][:], gt[:], ALU.add, [("hac", hb), ("gsb", 0)], [("hT", fc, G)])
              P.barrier()
              ckpt(5)

          with ExitStack() as s5:
              wo = sbt(s5, "wo", [128, 8, 1024], BF16)
              stgB = [sbt(s5, f"stgB{i}", [128, 2, 1024]) for i in range(2)]
              xres = [sbt(s5, f"xres{i}", [128, 1024]) for i in range(2)]
              rr = [sbt(s5, f"rr{i}", [128, 1024]) for i in range(2)]
              xf = [sbt(s5, f"xf{i}", [128, 8, 128]) for i in range(2)]
              lng = sbt(s5, "lng", [128, 1024]); lnb = sbt(s5, "lnb", [128, 1024])
              wr = sbt(s5, "wr", [128, 8, 36]); brt = sbt(s5, "brt", [128, 36])
              st_ = [sbt(s5, f"st{i}", [128, 2, 6]) for i in range(2)]
              mvv = [sbt(s5, f"mv{i}", [128, 2]) for i in range(2)]
              sml = [sbt(s5, f"sml{i}", [128, 16]) for i in range(2)]
              lg = [sbt(s5, f"lg{i}", [128, 36]) for i in range(2)]
              me = [sbt(s5, f"me{i}", [128, 32]) for i in range(2)]
              oh = [sbt(s5, f"oh{i}", [128, 2, 32]) for i in range(2)]
              top8 = [sbt(s5, f"top8{i}", [128, 8]) for i in range(2)]
              for c in range(4):
                  i = c % 2
                  P.dma("sp", stgB[i][:], w_out_d[c * 256:(c + 1) * 256, :].rearrange("(c p) n -> p c n", p=128), writes=[("stgB", i)])
                  cp("pool", wo[:, c * 2:(c + 1) * 2, :], stgB[i][:], [("stgB", i)], [("wo", c)])
              WO = [("wo", c) for c in range(4)]
              P.dma("sp", lng[:], ln_d[0].partition_broadcast(128), writes=["lng"])
              P.dma("sp", lnb[:], ln_d[1].partition_broadcast(128), writes=["lnb"])
              P.dma("sp", wr[:], w_r_d.rearrange("(c p) n -> p c n", p=128), writes=["wr"])
              P.dma("sp", brt[:], b_r_d.partition_broadcast(128), writes=["brt"])
              if True:
                  ckpt(60)
              def stA1(blk):
                  i = blk % 2
                  G = blk // 4
                  RR = [("rr", i, 0), ("rr", i, 1)]
                  LG = [("lg", i)]
                  s_ = sml[i]
                  SK = [("smr", i)]
                  P.dma("sp", xres[i][:], xo_d[blk * 128:(blk + 1) * 128, :], writes=[("xres", i)])
                  for ch in range(2):
                      bank = 2 * i + ch
                      for fc in range(8):
                          mm(ps[bank][:, :], hT[:, fc, blk * 128:(blk + 1) * 128], wo[:, fc, ch * 512:(ch + 1) * 512], fc == 0, fc == 7,
                             [("hT", fc, G)] + WO, [PSK[bank]])
                      stt("dve", rr[i][:, ch * 512:(ch + 1) * 512], xres[i][:, ch * 512:(ch + 1) * 512], ALPHA, ps[bank][:, :],
                          ALU.mult, ALU.add, [("xres", i), PSK[bank]], [("rr", i, ch)])
                      P.op("dve", lambda e: e.bn_stats(out=st_[i][:, ch, :], in_=rr[i][:, ch * 512:(ch + 1) * 512]), [("rr", i, ch)], [("st", i, ch)])
                  P.op("dve", lambda e: e.bn_aggr(out=mvv[i][:], in_=st_[i][:].rearrange("p a b -> p (a b)")), [("st", i, 0), ("st", i, 1)], [("mvv", i)])
                  act(sml[i][:, 0:1], mvv[i][:, 1:2], AF.Ln, [("mvv", i)], [("sml", i)], bias=LN_EPS)
                  act(sml[i][:, 0:1], sml[i][:, 0:1], AF.Exp, [("sml", i)], [("sml", i)], scale=-0.5)
                  RR = [("rr", i, 0), ("rr", i, 1)]
                  ts("dve", rr[i][:], rr[i][:], mvv[i][:, 0:1], ALU.subtract, RR + [("mvv", i), ("sml", i)], RR, s2=sml[i][:, 0:1], op1=ALU.mult)
                  tt("pool", rr[i][:], rr[i][:], lng[:], ALU.mult, RR + ["lng"], RR)
                  tt("pool", rr[i][:], rr[i][:], lnb[:], ALU.add, RR + ["lnb"], RR)
                  if debug:
                      P.dma("sp", dbg["x1"][blk * 128:(blk + 1) * 128, :], rr[i][:], reads=RR, writes=[("dbg_x1", blk)])
                  act(acc[:, blk, :], rr[i][:], AF.Copy, RR, [("acc", blk)], scale=ALPHA)

              def stA2(blk):
                  i = blk % 2
                  G = blk // 4
                  RR = [("rr", i, 0), ("rr", i, 1)]
                  LG = [("lg", i)]
                  s_ = sml[i]
                  SK = [("smr", i)]
                  for c in range(8):
                      bank = 4 + 2 * i + c // 4
                      tr(ps[bank][:, (c % 4) * 128:(c % 4 + 1) * 128], rr[i][:, c * 128:(c + 1) * 128], RR, [PSK[bank]])
                  for hf in range(2):
                      bank = 4 + 2 * i + hf
                      pv = ps[bank][:, :].rearrange("p (c t) -> p c t", c=4)
                      cp("dve", xf[i][:, hf * 4:(hf + 1) * 4, :], pv, [PSK[bank]], [("xf", i, hf)])
                      cp("act", x1T[:, hf * 4:(hf + 1) * 4, blk * 128:(blk + 1) * 128], xf[i][:, hf * 4:(hf + 1) * 4, :], [("xf", i, hf)], [("x1T", blk, hf)])
                  rb = 4 + 2 * i
                  for dc in range(8):
                      mm(ps[rb][:, 0:36], xf[i][:, dc, :], wr[:, dc, :], dc == 0, dc == 7, [("xf", i, 0), ("xf", i, 1), "wr"], [PSK[rb]])
                  LG = [("lg", i)]
                  tt("dve", lg[i][:], ps[rb][:, 0:36], brt[:], ALU.add, [PSK[rb], "brt"], LG)
                  s_ = sml[i]
                  SK = [("smr", i)]

              def stB(blk):
                  i = blk % 2
                  G = blk // 4
                  RR = [("rr", i, 0), ("rr", i, 1)]
                  LG = [("lg", i)]
                  s_ = sml[i]
                  SK = [("smr", i)]
                  tt("dve", s_[:, 12:13], lg[i][:, 0:1], lg[i][:, 1:2], ALU.max, LG, SK)
                  tt("dve", s_[:, 13:14], lg[i][:, 2:3], lg[i][:, 3:4], ALU.max, LG + SK, SK)
                  tt("dve", s_[:, 1:2], s_[:, 12:13], s_[:, 13:14], ALU.max, SK, SK)
                  ts("dve", s_[:, 2:3], s_[:, 1:2], -1.0, ALU.mult, SK, SK)
                  act(s_[:, 8:12], lg[i][:, 0:4], AF.Exp, LG + SK, SK, bias=s_[:, 2:3])
                  tt("dve", s_[:, 12:13], s_[:, 8:9], s_[:, 9:10], ALU.add, SK, SK)
                  tt("dve", s_[:, 13:14], s_[:, 10:11], s_[:, 11:12], ALU.add, SK, SK)
                  tt("dve", s_[:, 3:4], s_[:, 12:13], s_[:, 13:14], ALU.add, SK, SK)
                  P.op("dve", lambda e: e.reciprocal(out=s_[:, 4:5], in_=s_[:, 3:4]), SK, SK)
                  ts("dve", s_[:, 8:12], lg[i][:, 0:4], s_[:, 1:2], ALU.is_equal, LG + SK, SK)
                  ts("dve", s_[:, 8:12], s_[:, 8:12], 1.0e4, ALU.mult, SK, SK, s2=-1.0e4, op1=ALU.add)
                  for g in range(4):
                      ts("dve", me[i][:, g * 8:(g + 1) * 8], lg[i][:, 4 + g * 8:4 + (g + 1) * 8], s_[:, 8 + g:9 + g], ALU.add,
                         LG + SK, [("me", i)])
                  P.op("dve", lambda e: e.max(out=top8[i][:], in_=me[i][:]), [("me", i)], [("top8", i)])
                  ts("dve", oh[i][:, 0, :], me[i][:], top8[i][:, 0:1], ALU.is_equal, [("me", i), ("top8", i)], [("oh", i)])
                  ts("dve", oh[i][:, 1, :], me[i][:], top8[i][:, 1:2], ALU.is_equal, [("me", i), ("top8", i), ("oh", i)], [("oh", i)])
                  tt("dve", s_[:, 5:6], top8[i][:, 1:2], top8[i][:, 0:1], ALU.subtract, [("top8", i)] + SK, SK)
                  act(s_[:, 5:6], s_[:, 5:6], AF.Exp, SK, SK)
                  ts("dve", s_[:, 5:6], s_[:, 5:6], 1.0, ALU.add, SK, SK)
                  P.op("dve", lambda e: e.reciprocal(out=s_[:, 5:6], in_=s_[:, 5:6]), SK, SK)
                  tt("dve", s_[:, 6:7], s_[:, 5:6], s_[:, 4:5], ALU.mult, SK, SK)
                  tt("dve", s_[:, 7:8], s_[:, 4:5], s_[:, 6:7], ALU.subtract, SK, SK)
                  ts("dve", comb[:, blk, :], oh[i][:, 0, :], s_[:, 6:7], ALU.mult, [("oh", i)] + SK, [("comb", blk)])
                  stt("dve", comb[:, blk, :], oh[i][:, 1, :], s_[:, 7:8], comb[:, blk, :], ALU.mult, ALU.add, [("oh", i), ("comb", blk)] + SK,
                      [("comb", blk)])

              stA1(0)
              for blk in range(16):
                  if blk + 1 < 16:
                      stA1(blk + 1)
                  stA2(blk)
                  stB(blk)
              if debug:
                  P.dma("sp", dbg["comb"], comb[:], reads=[("comb", b) for b in range(16)], writes=["dbg_comb"])
              P.barrier()
              ckpt(6)

          with ExitStack() as s6:
              weg = [sbt(s6, f"weg{i}", [128, 8, 256], BF16) for i in range(2)]
              weu = [sbt(s6, f"weu{i}", [128, 8, 256], BF16) for i in range(2)]
              wed = [sbt(s6, f"wed{i}", [128, 2, 1024], BF16) for i in range(2)]
              stgE = [sbt(s6, f"stgE{i}", [128, 2048]) for i in range(3)]
              sg_ = [sbt(s6, f"sg{i}", [128, 512]) for i in range(2)]
              aT = [sbt(s6, f"aT{i}", [128, 2, 512], BF16) for i in range(2)]
              se = [0]

              def load_e(slot, e_):
                  for j, (src, dst) in enumerate(((w_eg, weg), (w_eu, weu))):
                      i = se[0] % 3; se[0] += 1
                      sv = stgE[i][:].rearrange("p (c n) -> p c n", c=8)
                      P.dma("sp", sv, src[e_].rearrange("(c p) n -> p c n", p=128), writes=[("stgE", i)])
                      cp("pool", dst[slot][:], sv, [("stgE", i)], [("we", slot, j)])
                  i = se[0] % 3; se[0] += 1
                  sv = stgE[i][:].rearrange("p (c n) -> p c n", c=2)
                  P.dma("sp", sv, w_ed[e_].rearrange("(c p) n -> p c n", p=128), writes=[("stgE", i)])
                  cp("pool", wed[slot][:], sv, [("stgE", i)], [("we", slot, 2)])

              load_e(0, 0)
              it = 0; yb = 0
              for e_ in range(NE):
                  slot = e_ % 2
                  if e_ + 1 < NE:
                      load_e(1 - slot, e_ + 1)
                  for G in range(4):
                      ai = it % 2; it += 1
                      for fc in range(2):
                          bg, bu = (0, 1) if fc == 0 else (2, 3)
                          for dc in range(8):
                              mm(ps[bg][:, :], weg[slot][:, dc, fc * 128:(fc + 1) * 128], x1T[:, dc, G * 512:(G + 1) * 512], dc == 0, dc == 7,
                                 [("we", slot, 0)], [PSK[bg]])
                          for dc in range(8):
                              mm(ps[bu][:, :], weu[slot][:, dc, fc * 128:(fc + 1) * 128], x1T[:, dc, G * 512:(G + 1) * 512], dc == 0, dc == 7,
                                 [("we", slot, 1)], [PSK[bu]])
                          act(sg_[fc][:], ps[bg][:, :], AF.Silu, [PSK[bg]], [("sg", fc)])
                          tt("dve", aT[ai][:, fc, :], sg_[fc][:], ps[bu][:, :], ALU.mult, [("sg", fc), PSK[bu]], [("aT", ai, fc)])
                      for b4 in range(4):
                          blk = G * 4 + b4
                          for ch in range(2):
                              bank = 4 + (yb % 4); yb += 1
                              for fc in range(2):
                                  mm(ps[bank][:, :], aT[ai][:, fc, b4 * 128:(b4 + 1) * 128], wed[slot][:, fc, ch * 512:(ch + 1) * 512], fc == 0, fc == 1,
                                     [("aT", ai, 0), ("aT", ai, 1), ("we", slot, 2)], [PSK[bank]])
                              stt("dve", acc[:, blk, ch * 512:(ch + 1) * 512], ps[bank][:, :], comb[:, blk, e_:e_ + 1], acc[:, blk, ch * 512:(ch + 1) * 512],
                                  ALU.mult, ALU.add, [PSK[bank], ("accw", blk, ch)], [("accw", blk, ch)])
              P.barrier()
              ckpt(7)

          with ExitStack() as s7:
              lng2 = sbt(s7, "lng2", [128, 1024]); lnb2 = sbt(s7, "lnb2", [128, 1024])
              ot = [sbt(s7, f"ot{i}", [128, 1024]) for i in range(2)]
              st2 = [sbt(s7, f"st2{i}", [128, 2, 6]) for i in range(2)]
              mv2 = [sbt(s7, f"mv2{i}", [128, 2]) for i in range(2)]
              rs2 = [sbt(s7, f"rs2{i}", [128, 1]) for i in range(2)]
              P.dma("sp", lng2[:], ln_d[2].partition_broadcast(128), writes=["lng2"])
              P.dma("sp", lnb2[:], ln_d[3].partition_broadcast(128), writes=["lnb2"])
              outk = []
              for blk in range(16):
                  i = blk % 2
                  for ch in range(2):
                      P.op("dve", lambda e: e.bn_stats(out=st2[i][:, ch, :], in_=acc[:, blk, ch * 512:(ch + 1) * 512]), [], [("st2", i, ch)])
                  P.op("dve", lambda e: e.bn_aggr(out=mv2[i][:], in_=st2[i][:].rearrange("p a b -> p (a b)")), [("st2", i, 0), ("st2", i, 1)], [("mv2", i)])
                  act(rs2[i][:], mv2[i][:, 1:2], AF.Ln, [("mv2", i)], [("rs2", i)], bias=LN_EPS)
                  act(rs2[i][:], rs2[i][:], AF.Exp, [("rs2", i)], [("rs2", i)], scale=-0.5)
                  ts("dve", ot[i][:], acc[:, blk, :], mv2[i][:, 0:1], ALU.subtract, [("mv2", i), ("rs2", i)], [("ot", i)], s2=rs2[i][:, 0:1], op1=ALU.mult)
                  tt("pool", ot[i][:], ot[i][:], lng2[:], ALU.mult, [("ot", i), "lng2"], [("ot", i)])
                  tt("pool", ot[i][:], ot[i][:], lnb2[:], ALU.add, [("ot", i), "lnb2"], [("ot", i)])
                  P.dma("sp", out_d[blk * 128:(blk + 1) * 128, :], ot[i][:], reads=[("ot", i)], writes=[("out", blk)])
                  outk.append(("out", blk))
              P.finish(outk + [k for k in P.bufs if isinstance(k, str) and k.startswith("dbg")] +
                       [k for k in P.bufs if isinstance(k, tuple) and isinstance(k[0], str) and k[0].startswith("dbg")])
              P.barrier()
    except _Stop:
        pass
    return nc


def _blocks(p):
    own, oth = [], []
    for j in range(8):
        if p == 0:
            own += [4 * j, 4 * j + 3]; oth += [4 * j + 1, 4 * j + 2]
        else:
            own += [4 * j + 1, 4 * j + 2]; oth += [4 * j, 4 * j + 3]
    return own, oth


def _tok(blocks):
    return np.concatenate([np.arange(b * 128, (b + 1) * 128) for b in blocks])


_NC_CACHE = {}


def _prepare(x, mem, w_in, b_forget, b_gates, lambda_q1, lambda_k1, lambda_q2, lambda_k2,
           diff_subln_g, w_mem_kv, w_branch_fox, w_branch_diff, w_branch_mem, w_out,
           ln1_g, ln1_b, w_router_group, b_router_group, w_router_expert, b_router_expert,
           w_expert_gate, w_expert_up, w_expert_down, ln2_g, ln2_b):
    f = lambda a: np.ascontiguousarray(np.asarray(a), dtype=np.float32)
    x = f(x); mem = f(mem)
    shared = {
        "w_in": f(w_in[0]), "w_mkv": f(w_mem_kv[0]),
        "w_bf": f(w_branch_fox[0]), "w_bd": f(w_branch_diff[0]), "w_bm": f(w_branch_mem[0]),
        "w_out": f(w_out[0]),
        "w_r": f(np.concatenate([np.asarray(w_router_group[0]), np.asarray(w_router_expert[0])], axis=1)),
        "w_eg": f(w_expert_gate[0]), "w_eu": f(w_expert_up[0]), "w_ed": f(w_expert_down[0]),
        "nbf": f(-np.asarray(b_forget[0]).reshape(8, 1)),
        "bgt": f(np.asarray(b_gates[0]).reshape(24, 128).T),
        "lam4": f(np.stack([np.asarray(lambda_q1[0]), np.asarray(lambda_k1[0]), np.asarray(lambda_q2[0]), np.asarray(lambda_k2[0])])),
        "subln_g": f(np.asarray(diff_subln_g[0]).reshape(128, 1)),
        "ln1_g": f(ln1_g[0]), "ln1_b": f(ln1_b[0]), "ln2_g": f(ln2_g[0]), "ln2_b": f(ln2_b[0]),
        "b_r": f(np.concatenate([np.asarray(b_router_group[0]), np.asarray(b_router_expert[0])])),
    }
    rst = np.ones((8, S), np.float32); rst[:, ::128] = 0.0
    shared["rst"] = rst
    slopes = [2.0 ** (-8.0 * (h + 1) / 4) for h in range(4)]

    def hi_lo(v):
        hi = np.floor(v / 16.0) * 16.0
        return hi, v - hi

    in_maps = []
    toks = []
    for c in range(NCORES):
        b, p = c // 2, c % 2
        own, oth = _blocks(p)
        to, tr_ = _tok(own), _tok(oth)
        toks.append((b, to))
        m = dict(shared)
        m["xTo"] = f(x[b, to, :].T); m["xTr"] = f(x[b, tr_, :].T); m["xo"] = f(x[b, to, :])
        m["memT"] = f(mem[b].T)
        par = np.zeros((128, 2), np.float32); par[:, 0] = 1.0 - p; par[:, 1] = p
        m["par"] = par
        pos_k = np.concatenate([to, tr_]).astype(np.float64)
        pos_q = to.astype(np.float64)
        dq = np.ones((4, 4, T), np.float32); dk = np.ones((4, 4, S), np.float32)
        for h in range(4):
            hq, lq = hi_lo(pos_q); hk, lk = hi_lo(pos_k)
            dq[h, 0] = -slopes[h] * hq; dq[h, 1] = -slopes[h] * lq
            dk[h, 2] = slopes[h] * hk; dk[h, 3] = slopes[h] * lk
        m["dqaug"] = dq; m["dkaug"] = dk
        in_maps.append(m)
    return in_maps, toks


def kernel(**inputs):
    debug = bool(int(os.environ.get("MK_DEBUG", "0")))
    in_maps, toks = _prepare(**inputs)
    key = debug
    if key not in _NC_CACHE:
        _NC_CACHE[key] = build_program(debug)
    nc = _NC_CACHE[key]
    res = run_bass_kernel_spmd(nc, in_maps, core_ids=list(range(NCORES)))
    out = np.empty((4, S, D), np.float32)
    for c in range(NCORES):
        b, to = toks[c]
        out[b, to, :] = res.results[c]["out"]
    if debug:
        kernel.debug_results = res.results
        kernel.toks = toks
    return out
```
